# Optimizing a Trainium2 kernel written in Bass

```python
import math
import jax, jax.numpy as jnp
from jax import lax
import numpy as np

D_MODEL = 1024
BATCH = 16
SEQ = 2048
DEPTH = 1

HEAD_DIM = 64
N_ATTN_HEADS = 8
N_HGRN_HEADS = 8
ATTN_WIDTH = N_ATTN_HEADS * HEAD_DIM
HGRN_WIDTH = N_HGRN_HEADS * HEAD_DIM
MIX_WIDTH = ATTN_WIDTH + HGRN_WIDTH
IN_PROJ_WIDTH = 3 * ATTN_WIDTH + 4 * HGRN_WIDTH
DILATED_CONFIGS = ((128, 1), (512, 4), (2048, 16))
ATTN_BLOCK = 128
HGRN_CHUNK = 32
N_GROUPS = 4
EXPERTS_PER_GROUP = 8
N_EXPERTS = N_GROUPS * EXPERTS_PER_GROUP
EXPERT_TOP_K = 2
D_EXPERT = 512
MOE_BLOCK = 128
EPS = 1e-6

kernel_name = "hybrid_dilated_attn_hgrn2_hmoe"


def rmsnorm(x, g):
    xf = x.astype(jnp.float32)
    y = xf * lax.rsqrt(jnp.mean(xf * xf, axis=-1, keepdims=True) + EPS)
    return (y * g.astype(jnp.float32)).astype(x.dtype)


def alibi_slopes(n_heads):
    return 2.0 ** (-8.0 * jnp.arange(1, n_heads + 1, dtype=jnp.float32) / n_heads)


def dilated_branch(q, k, v, slopes, window, dilation):
    B, S, H, Dh = q.shape
    n_keys = window // dilation
    assert n_keys <= ATTN_BLOCK
    U = S // dilation
    nb = -(-U // ATTN_BLOCK)
    Up = nb * ATTN_BLOCK

    def to_blocks(t):
        t = t.reshape(B, U, dilation, H, Dh).transpose(0, 2, 1, 3, 4)
        t = jnp.pad(t, ((0, 0), (0, 0), (0, Up - U), (0, 0), (0, 0)))
        return t.reshape(B, dilation, nb, ATTN_BLOCK, H, Dh)

    def from_blocks(t):
        rest = t.shape[4:]
        t = t.reshape((B, dilation, Up) + rest)[:, :, :U]
        return jnp.swapaxes(t, 1, 2).reshape((B, S) + rest)

    def with_prev(t):
        prev = jnp.pad(t, ((0, 0), (0, 0), (1, 0), (0, 0), (0, 0), (0, 0)))[:, :, :-1]
        return jnp.concatenate([prev, t], axis=3)

    qb = to_blocks(q)
    kk = with_prev(to_blocks(k))
    vv = with_prev(to_blocks(v))
    scores = jnp.einsum('bgnqhd,bgnkhd->bgnhqk', qb, kk,
                        preferred_element_type=jnp.float32) / math.sqrt(Dh)
    qi = jnp.arange(ATTN_BLOCK)[:, None]
    ki = jnp.arange(2 * ATTN_BLOCK)[None, :]
    sub_dist = qi + ATTN_BLOCK - ki
    band = (sub_dist >= 0) & (sub_dist <= n_keys)
    key_pos = jnp.arange(nb)[:, None, None] * ATTN_BLOCK + ki[None] - ATTN_BLOCK
    valid = band[None] & (key_pos >= 0)
    bias = -slopes[:, None, None] * (sub_dist * dilation).astype(jnp.float32)[None]
    scores = jnp.where(valid[None, None, :, None], scores + bias[None, None, None], -jnp.inf)
    lse = jax.nn.logsumexp(scores, axis=-1)
    p = jnp.exp(scores - lse[..., None])
    o = jnp.einsum('bgnhqk,bgnkhd->bgnqhd', p, vv.astype(jnp.float32))
    return from_blocks(o), from_blocks(jnp.swapaxes(lse, -1, -2))


def dilated_attention(q, k, v):
    slopes = alibi_slopes(q.shape[2])
    outs, lses = [], []
    for window, dilation in DILATED_CONFIGS:
        o_c, lse_c = dilated_branch(q, k, v, slopes, window, dilation)
        outs.append(o_c)
        lses.append(lse_c)
    w = jax.nn.softmax(jnp.stack(lses, axis=0), axis=0)
    return jnp.sum(w[..., None] * jnp.stack(outs, axis=0), axis=0)


def hgrn2(q, f_raw, i_in, lb):
    B, S, H, Dk = q.shape
    Dv = i_in.shape[-1]
    C = HGRN_CHUNK
    nC = S // C
    f32 = jnp.float32
    q, f_raw, i_in = q.astype(f32), f_raw.astype(f32), i_in.astype(f32)
    lb = lb.reshape(H, Dk)
    log_f = jnp.log(lb + (1.0 - lb) * jax.nn.sigmoid(f_raw))
    key = (1.0 - lb) * jax.nn.sigmoid(-f_raw)
    q = jax.nn.silu(q)
    rs = lambda t: t.reshape(B, nC, C, H, t.shape[-1])
    qc, kc, vc, lfc = rs(q), rs(key), rs(i_in), rs(log_f)
    b = jnp.cumsum(lfc, axis=2)
    b_last = b[:, :, -1:]
    q_in = qc * jnp.exp(b)
    k_in = kc * jnp.exp(-b)
    k_end = kc * jnp.exp(b_last - b)
    causal = jnp.tril(jnp.ones((C, C), dtype=bool))
    A = jnp.where(causal, jnp.einsum('bnthk,bnshk->bnhts', q_in, k_in), 0.0)
    o_intra = jnp.einsum('bnhts,bnshv->bnthv', A, vc)
    dS = jnp.einsum('bnshk,bnshv->bnhkv', k_end, vc)
    decay = jnp.exp(b_last[:, :, 0])

    def step(S_prev, inp):
        dec, ds = inp
        return dec[..., None] * S_prev + ds, S_prev

    S0 = jnp.zeros((B, H, Dk, Dv), f32)
    _, S_prevs = lax.scan(step, S0, (jnp.swapaxes(decay, 0, 1), jnp.swapaxes(dS, 0, 1)))
    S_prevs = jnp.swapaxes(S_prevs, 0, 1)
    o_inter = jnp.einsum('bnthk,bnhkv->bnthv', q_in, S_prevs)
    return (o_intra + o_inter).reshape(B, S, H, Dv)


def hier_moe(h, w_group, b_group, w_router, b_router, w_gate, w_up, w_down):
    B, S, D = h.shape
    N = B * S
    f32 = jnp.float32
    xf = h.reshape(N, D)
    grp_logits = (xf @ w_group).astype(f32) + b_group.astype(f32)
    p_grp = jax.nn.softmax(grp_logits, axis=-1)
    g_sel = jnp.argmax(grp_logits, axis=-1).astype(jnp.int32)
    w_g = jnp.take_along_axis(p_grp, g_sel[:, None], axis=1)
    exp_logits = ((xf @ w_router).astype(f32) + b_router.astype(f32)).reshape(N, N_GROUPS, EXPERTS_PER_GROUP)
    sel_logits = jnp.take_along_axis(exp_logits, g_sel[:, None, None], axis=1)[:, 0]
    top_vals, top_idx = lax.top_k(sel_logits, EXPERT_TOP_K)
    gate = w_g * jax.nn.softmax(top_vals, axis=-1)
    eid = (g_sel[:, None] * EXPERTS_PER_GROUP + top_idx).reshape(-1).astype(jnp.int32)
    tok = jnp.repeat(jnp.arange(N, dtype=jnp.int32), EXPERT_TOP_K)
    A_n = N * EXPERT_TOP_K
    order = jnp.argsort(eid)
    eid_s, tok_s, gate_s = eid[order], tok[order], gate.reshape(-1)[order]
    counts = jnp.bincount(eid, length=N_EXPERTS).astype(jnp.int32)
    starts = jnp.cumsum(counts) - counts
    padded = ((counts + MOE_BLOCK - 1) // MOE_BLOCK) * MOE_BLOCK
    pad_ends = jnp.cumsum(padded)
    pad_starts = pad_ends - padded
    dest = pad_starts[eid_s] + jnp.arange(A_n, dtype=jnp.int32) - starts[eid_s]
    P = (-(-A_n // MOE_BLOCK)) * MOE_BLOCK + N_EXPERTS * MOE_BLOCK
    n_blocks = P // MOE_BLOCK
    buf_tok = jnp.full((P,), N, jnp.int32).at[dest].set(tok_s)
    buf_gate = jnp.zeros((P,), f32).at[dest].set(gate_s)
    block_expert = jnp.minimum(
        jnp.searchsorted(pad_ends, jnp.arange(n_blocks, dtype=jnp.int32) * MOE_BLOCK, side='right'),
        N_EXPERTS - 1).astype(jnp.int32)
    x_pad = jnp.concatenate([xf, jnp.zeros((1, D), xf.dtype)], axis=0)

    def run_block(args):
        e, toks, gts = args
        xb = x_pad[toks]
        hid = jax.nn.silu(xb @ w_gate[e]) * (xb @ w_up[e])
        return ((hid @ w_down[e]) * gts[:, None].astype(xb.dtype)).astype(h.dtype)

    y = lax.map(run_block, (block_expert, buf_tok.reshape(n_blocks, MOE_BLOCK),
                            buf_gate.reshape(n_blocks, MOE_BLOCK)))
    out = jnp.zeros((N + 1, D), h.dtype).at[buf_tok].add(y.reshape(P, D))[:N]
    return out.reshape(B, S, D)


def setup_inputs(seed: int = 0) -> dict:
    key = jax.random.key(seed)
    ks = jax.random.split(key, 16)
    f32 = jnp.float32
    nrm = lambda k, shape, scale: jax.random.normal(k, shape, f32) * scale
    L = DEPTH
    return {
        "x": jax.random.normal(ks[0], (BATCH, SEQ, D_MODEL), f32),
        "norm1_g": 1.0 + nrm(ks[1], (L, D_MODEL), 0.05),
        "w_in": nrm(ks[2], (L, D_MODEL, IN_PROJ_WIDTH), D_MODEL ** -0.5),
        "attn_norm_g": 1.0 + nrm(ks[3], (L, ATTN_WIDTH), 0.05),
        "hgrn_gamma": nrm(ks[4], (L + 1, HGRN_WIDTH), 0.1),
        "hgrn_norm_g": 1.0 + nrm(ks[5], (L, HGRN_WIDTH), 0.05),
        "w_out": nrm(ks[6], (L, MIX_WIDTH, D_MODEL), MIX_WIDTH ** -0.5),
        "norm2_g": 1.0 + nrm(ks[7], (L, D_MODEL), 0.05),
        "w_group": nrm(ks[8], (L, D_MODEL, N_GROUPS), D_MODEL ** -0.5),
        "b_group": nrm(ks[9], (L, N_GROUPS), 0.01),
        "w_router": nrm(ks[10], (L, D_MODEL, N_EXPERTS), D_MODEL ** -0.5),
        "b_router": nrm(ks[11], (L, N_EXPERTS), 0.01),
        "w_gate": nrm(ks[12], (L, N_EXPERTS, D_MODEL, D_EXPERT), D_MODEL ** -0.5),
        "w_up": nrm(ks[13], (L, N_EXPERTS, D_MODEL, D_EXPERT), D_MODEL ** -0.5),
        "w_down": nrm(ks[14], (L, N_EXPERTS, D_EXPERT, D_MODEL), D_EXPERT ** -0.5),
        "norm_f_g": 1.0 + nrm(ks[15], (D_MODEL,), 0.05),
    }


def reference(x, norm1_g, w_in, attn_norm_g, hgrn_gamma, hgrn_norm_g, w_out, norm2_g,
              w_group, b_group, w_router, b_router, w_gate, w_up, w_down, norm_f_g):
    B, S, _ = x.shape
    split_at = [ATTN_WIDTH, 2 * ATTN_WIDTH, 3 * ATTN_WIDTH,
                3 * ATTN_WIDTH + HGRN_WIDTH, 3 * ATTN_WIDTH + 2 * HGRN_WIDTH,
                3 * ATTN_WIDTH + 3 * HGRN_WIDTH]
    lower_bounds = jnp.cumsum(jax.nn.softmax(hgrn_gamma.astype(jnp.float32), axis=0), axis=0)
    for layer in range(DEPTH):
        h = rmsnorm(x, norm1_g[layer])
        proj = h @ w_in[layer]
        qa, ka, va, qh, fh, ih, gh = jnp.split(proj, split_at, axis=-1)
        heads = lambda t, n: t.reshape(B, S, n, HEAD_DIM)
        o_attn = dilated_attention(heads(qa, N_ATTN_HEADS), heads(ka, N_ATTN_HEADS),
                                   heads(va, N_ATTN_HEADS)).reshape(B, S, ATTN_WIDTH)
        y_attn = rmsnorm(o_attn, attn_norm_g[layer])
        o_h = hgrn2(heads(qh, N_HGRN_HEADS), heads(fh, N_HGRN_HEADS), heads(ih, N_HGRN_HEADS),
                    lower_bounds[layer])
        o_h = rmsnorm(o_h, hgrn_norm_g[layer].reshape(N_HGRN_HEADS, HEAD_DIM)).reshape(B, S, HGRN_WIDTH)
        y_hgrn = o_h * jax.nn.silu(gh.astype(jnp.float32))
        mixed = jnp.concatenate([y_attn, y_hgrn], axis=-1).astype(x.dtype)
        x = x + mixed @ w_out[layer]
        h2 = rmsnorm(x, norm2_g[layer])
        x = x + hier_moe(h2, w_group[layer], b_group[layer], w_router[layer], b_router[layer],
                         w_gate[layer], w_up[layer], w_down[layer])
    return rmsnorm(x, norm_f_g)
```

```python
import numpy as np
import ml_dtypes
from contextlib import ExitStack
import concourse.bass as bass
import concourse.mybir as mybir
from concourse.bass_utils import run_bass_kernel_spmd

F32 = mybir.dt.float32
BF16 = mybir.dt.bfloat16
I32 = mybir.dt.int32
AF = mybir.ActivationFunctionType
ALU = mybir.AluOpType
AX = mybir.AxisListType

COMPUTE = ("pe", "act", "dve", "pool")
QUEUES = ("sp", "act", "pool")
KSLOTS = {"sp": 8, "act": 8, "pool": 8}
ENGS = ("pe", "act", "dve", "pool", "sp")


class Buf:
    __slots__ = ("name", "lw", "rd")

    def __init__(self, name):
        self.name = name
        self.lw = None
        self.rd = []


class Op:
    __slots__ = ("eng", "fn", "tl", "ord", "waits", "clock", "is_dma", "idx")


class Prog:
    def __init__(self, nc, same_engine_sync=("act", "dve", "pool")):
        self.nc = nc
        self.ops = []
        self.eng_ops = {e: [] for e in ENGS}
        self.known = {e: {} for e in ENGS}
        self.cnt = {e: 0 for e in COMPUTE}
        self.dcnt = {q: 0 for q in QUEUES}
        self.last = {}
        self.same_sync = set(same_engine_sync)
        self.nbuf = 0

    def buf(self, name=None):
        self.nbuf += 1
        return Buf(name or f"b{self.nbuf}")

    def _need(self, X, tl, o, clock, waits):
        kn = self.known[X]
        if kn.get(tl, 0) >= o:
            return
        waits.append((tl, o))
        for k, v in clock.items():
            if kn.get(k, 0) < v:
                kn[k] = v

    def op(self, eng, fn, reads=(), writes=(), dma=False):
        o = Op()
        o.eng, o.fn, o.is_dma, o.idx = eng, fn, dma, len(self.ops)
        deps = set()
        for b in reads:
            if b.lw is not None:
                deps.add(b.lw)
        for b in writes:
            if b.lw is not None:
                deps.add(b.lw)
            deps.update(b.rd)
        waits = []
        if dma:
            n = self.dcnt[eng]
            self.dcnt[eng] = n + 1
            K = KSLOTS[eng]
            o.tl = ("q", eng, n % K)
            o.ord = n // K + 1
            if o.ord > 1:
                po, pc = self.last[o.tl]
                self._need(eng, o.tl, po, pc, waits)
        else:
            self.cnt[eng] += 1
            o.tl = eng
            o.ord = self.cnt[eng]
        for j in sorted(deps, reverse=True):
            d = self.ops[j]
            if d.tl == eng and not dma and eng not in self.same_sync:
                continue
            self._need(eng, d.tl, d.ord, d.clock, waits)
        o.waits = waits
        ck = dict(self.known[eng])
        ck[o.tl] = o.ord
        o.clock = ck
        self.last[o.tl] = (o.ord, ck)
        for b in reads:
            b.rd.append(o.idx)
        for b in writes:
            b.lw = o.idx
            b.rd = []
        self.ops.append(o)
        self.eng_ops[eng].append(o)
        return o

    def _sync_all(self, engines, skip_bg=False):
        lasts = dict(self.last)
        if skip_bg:
            lasts = {tl: v for tl, v in lasts.items() if not (isinstance(tl, tuple) and tl[1] == "act")}
        for e in engines:
            o = Op()
            o.eng, o.fn, o.is_dma, o.idx = e, None, False, len(self.ops)
            waits = []
            for tl, (od, ck) in lasts.items():
                self._need(e, tl, od, ck, waits)
            o.waits = waits
            o.tl, o.ord = None, 0
            o.clock = dict(self.known[e])
            self.ops.append(o)
            self.eng_ops[e].append(o)

    def barrier(self):
        self._sync_all(ENGS, skip_bg=True)

    def final_wait(self, eng="sp"):
        self._sync_all((eng,))

    def emit(self, stack):
        nc = self.nc
        waited = {e: set() for e in COMPUTE}
        for o in self.ops:
            for tl, od in o.waits:
                if isinstance(tl, str):
                    waited[tl].add(od)
        rank = {e: {od: i + 1 for i, od in enumerate(sorted(waited[e]))} for e in COMPUTE}
        sems = {}
        for e in COMPUTE:
            sems[e] = stack.enter_context(nc.semaphore(f"s_{e}"))
        for q in QUEUES:
            for k in range(KSLOTS[q]):
                sems[("q", q, k)] = stack.enter_context(nc.semaphore(f"s_{q}{k}"))
        self.stats = {e: [len(self.eng_ops[e]), 0] for e in self.eng_ops}
        block = stack.enter_context(nc.Block())
        engmap = {"pe": "tensor", "act": "scalar", "dve": "vector", "pool": "gpsimd", "sp": "sync"}

        def make(e):
            def body(engine):
                for o in self.eng_ops[e]:
                    for tl, od in o.waits:
                        if isinstance(tl, str):
                            engine.wait_ge(sems[tl], rank[tl][od])
                        else:
                            engine.wait_ge(sems[tl], 16 * od)
                        self.stats[e][1] += 1
                    if o.fn is None:
                        continue
                    ins = o.fn(engine)
                    if o.is_dma:
                        ins.then_inc(sems[o.tl], 16)
                    elif o.ord in rank[o.tl]:
                        ins.then_inc(sems[o.tl], 1)
            return body

        for e in ENGS:
            getattr(block, engmap[e])(make(e))


class Arena:
    def __init__(self, ap, words):
        self.ap = ap
        self.words = words
        self.top = 0

    def alloc(self, n_elems, dtype=F32):
        bpe = 4 if dtype in (F32, I32) else 2
        w = (n_elems * bpe + 3) // 4
        w = (w + 15) // 16 * 16
        assert self.top + w <= self.words, f"arena overflow {self.top}+{w}>{self.words}"
        v = self.ap[:, self.top:self.top + w]
        self.top += w
        if dtype != F32:
            v = v.bitcast(dtype)
        return v[:, 0:n_elems]


NCORES = 8
S = 2048
NSEQ = 2
T = NSEQ * S
NT = T // 128
D = 1024
NE = 32
CAP = 384
NSLOT = NE * CAP
TRASH = NSLOT
EPS = 1e-6
DILS = (1, 4, 16)
NEG = -30000.0


def tokslice(c, vb):
    dil = DILS[c]
    if c == 0:
        return slice(128 * vb, 128 * vb + 128, 1)
    if c == 1:
        r, n = vb // 4, vb % 4
        st = 4 * 128 * n + r
        return slice(st, st + 4 * 127 + 1, 4)
    return slice(vb, vb + 16 * 127 + 1, 16)


def prev_vb(c, vb):
    if c == 0:
        return vb - 1 if vb > 0 else None
    if c == 1:
        return vb - 1 if vb % 4 > 0 else None
    return None


def host_consts():
    bf = ml_dtypes.bfloat16
    k = np.arange(128)[:, None].astype(np.float64)
    q = np.arange(128)[None, :].astype(np.float64)
    bt = np.zeros((128, 8, 3, 2, 128), np.float32)
    for h in range(8):
        s = 2.0 ** (-(h + 1))
        for c, dil in enumerate(DILS):
            dg = np.where(q >= k, -s * dil * (q - k), NEG)
            of = np.where(k >= q, -s * dil * (q + 128 - k), NEG)
            bt[:, h, c, 0, :] = dg
            bt[:, h, c, 1, :] = of
    cons = {}
    cons["btab"] = bt.reshape(128, 48 * 128).astype(bf)
    cons["ident_bf"] = np.eye(128, dtype=np.float32).astype(bf)
    cons["ident32"] = np.eye(128, dtype=np.float32)
    cm = (k <= q).astype(np.float32)
    cons["cmask"] = np.concatenate([cm, cm], axis=1).astype(np.float32)
    sm = np.ones((128, 512), np.float32)
    sm[:, 0::128] = 0.0
    cons["scanmask"] = sm
    bd = np.zeros((128, 128), np.float32)
    bd[0:64, 0:64] = 1.0
    bd[64:128, 64:128] = 1.0
    cons["bd_bf"] = bd.astype(bf)
    cons["ones_bf"] = np.ones((128, 128), np.float32).astype(bf)
    cons["tri_bf"] = (k < q).astype(np.float32).astype(bf)
    cons["ebase"] = np.broadcast_to((np.arange(NE) * CAP).astype(np.float32)[None, :], (128, NE)).copy()
    cons["zeros_bf"] = np.zeros((NSLOT + 128, D), bf)
    cons["zeros32"] = np.zeros((128, D), np.float32)
    return cons


def build():
    nc = bass.Bass("TRN2", target_bir_lowering=False)
    P = Prog(nc)
    st = ExitStack()

    def din(name, shape, dt=F32):
        return nc.dram_tensor(name, list(shape), dt, kind="ExternalInput").ap()

    x_d = din("x", [T, D])
    win_d = din("w_in", [D, 3584])
    wout_d = din("w_out", [D, D])
    g1_d = din("g1bc", [128, D])
    g2_d = din("g2bc", [128, D])
    gf_d = din("gfbc", [128, D])
    ga_d = din("ga", [128, 4])
    gh_d = din("gh", [128, 4])
    gam_d = din("gam", [128, 8])
    wr_d = din("wr", [128, 8, 36])
    br_d = din("brbc", [128, 36])
    wg_d = din("w_gate", [NE, D, 512])
    wu_d = din("w_up", [NE, D, 512])
    wd_d = din("w_down", [NE, 512, D])
    cshape = {"btab": ([128, 48 * 128], BF16), "ident_bf": ([128, 128], BF16), "ident32": ([128, 128], F32),
              "cmask": ([128, 256], F32), "scanmask": ([128, 512], F32), "bd_bf": ([128, 128], BF16),
              "ones_bf": ([128, 128], BF16), "tri_bf": ([128, 128], BF16), "ebase": ([128, NE], F32),
              "zeros_bf": ([NSLOT + 128, D], BF16), "zeros32": ([128, D], F32)}
    cd = {k: din("c_" + k, v[0], v[1]) for k, v in cshape.items()}
    out_d = nc.dram_tensor("out", [T, D], F32, kind="ExternalOutput").ap()
    x1s = nc.dram_tensor("x1s", [T, D], F32, kind="Internal").ap()
    h2s = nc.dram_tensor("h2s", [T, D], BF16, kind="Internal").ap()
    xg = nc.dram_tensor("xg", [NSLOT + 128, D], BF16, kind="Internal").ap()
    ybuf = nc.dram_tensor("ybuf", [NSLOT + 128, D], BF16, kind="Internal").ap()
    b_x1s = [P.buf() for _ in range(NT)]
    b_h2s = [P.buf() for _ in range(NT)]
    vsd = [nc.dram_tensor(f"vsd{i}", [S, 128], BF16, kind="Internal").ap() for i in range(2)]
    b_vs = [P.buf() for _ in range(2)]
    b_xg = P.buf("xg")
    b_xgz = [P.buf() for _ in range(4)]
    b_xge = [P.buf() for _ in range(NE)]
    b_ybuf = P.buf("ybuf")
    b_out = P.buf("out")

    AW = 53200
    arena_t = st.enter_context(nc.sbuf_tensor("arena", [128, AW], F32))
    A = Arena(arena_t[:, :], AW)
    pbank = [st.enter_context(nc.psum_tensor(f"pb{i}", [128, 512], F32)) for i in range(8)]
    pbuf = [P.buf(f"pb{i}") for i in range(8)]

    def PS(i, dt=F32):
        v = pbank[i][:, :]
        return v.bitcast(BF16) if dt == BF16 else v

    rot_state = {}

    def rot(name, banks):
        i = rot_state.get(name, 0)
        rot_state[name] = i + 1
        return banks[i % len(banks)]

    def dma(q, out, in_, reads=(), writes=()):
        P.op(q, lambda e: e.dma_start(out=out, in_=in_), reads, writes, dma=True)

    def mm(out, lhsT, rhs, start, stop, reads, writes):
        P.op("pe", lambda e: e.matmul(out, lhsT=lhsT, rhs=rhs, start=start, stop=stop), reads, writes)

    def tr(out, in_, ident, reads, writes):
        P.op("pe", lambda e: e.transpose(out=out, in_=in_, identity=ident), reads, writes)

    def act(out, in_, func, reads, writes, bias=None, scale=None, accum=None):
        kw = {}
        if bias is not None:
            kw["bias"] = bias
        if scale is not None:
            kw["scale"] = scale
        if accum is not None:
            kw["accum_out"] = accum
        P.op("act", lambda e: e.activation(out=out, in_=in_, func=func, **kw), reads, writes)

    def tt(eng, out, in0, in1, op, reads, writes):
        P.op(eng, lambda e: e.tensor_tensor(out=out, in0=in0, in1=in1, op=op), reads, writes)

    def ts(eng, out, in0, s1, s2, op0, op1, reads, writes):
        if s2 is None:
            P.op(eng, lambda e: e.tensor_scalar(out=out, in0=in0, scalar1=s1, scalar2=None, op0=op0), reads, writes)
        else:
            P.op(eng, lambda e: e.tensor_scalar(out=out, in0=in0, scalar1=s1, scalar2=s2, op0=op0, op1=op1), reads, writes)

    def stt(eng, out, in0, scalar, in1, op0, op1, reads, writes):
        P.op(eng, lambda e: e.scalar_tensor_tensor(out=out, in0=in0, scalar=scalar, in1=in1, op0=op0, op1=op1),
             reads, writes)

    def cp(eng, out, in_, reads, writes):
        if eng == "act":
            act(out, in_, AF.Copy, reads, writes)
        else:
            P.op(eng, lambda e: e.tensor_copy(out=out, in_=in_), reads, writes)

    def rsqrt_mean(out, in_, n, reads, writes):
        act(out, in_, AF.Ln, reads, writes, bias=epsc[:, 0:1], scale=1.0 / n)
        act(out, out, AF.Exp, writes, writes, scale=-0.5)

    C = {}
    bC = P.buf("consts")
    for k, (shp, dt) in cshape.items():
        if k in ("zeros_bf", "btab", "zeros32"):
            continue
        C[k] = A.alloc(shp[1], dt)
        dma("sp", C[k], cd[k], writes=[bC])
    epsc = A.alloc(1)
    P.op("pool", lambda e: e.memset(epsc, EPS), writes=[bC])
    g1bc = A.alloc(D); dma("sp", g1bc, g1_d, writes=[bC])
    ga = A.alloc(4); dma("sp", ga, ga_d, writes=[bC])
    gh = A.alloc(4); dma("sp", gh, gh_d, writes=[bC])
    gam = A.alloc(8); dma("sp", gam, gam_d, writes=[bC])
    lb = A.alloc(4); oml = A.alloc(4); noml = A.alloc(4)
    tt("dve", lb, gam[:, 0:4], gam[:, 4:8], ALU.subtract, [bC], [bC])
    act(lb, lb, AF.Sigmoid, [bC], [bC])
    ts("dve", oml, lb, -1.0, 1.0, ALU.mult, ALU.add, [bC], [bC])
    ts("dve", noml, oml, -1.0, None, ALU.mult, None, [bC], [bC])
    Lall = A.alloc(NT * 36)
    bL = P.buf("Lall")
    top_persist = A.top

    win = A.alloc(8 * 3584, BF16); bwin = P.buf("win")
    win3 = win.rearrange("p (c n) -> p c n", c=8)
    g2bc = A.alloc(D); dma("sp", g2bc, g2_d, writes=[bC])
    wr = A.alloc(8 * 36); dma("sp", wr, wr_d.rearrange("p c n -> p (c n)"), writes=[bC])
    wr3 = wr.rearrange("p (c n) -> p c n", c=8)
    brbc = A.alloc(36); dma("sp", brbc, br_d, writes=[bC])
    wrhi = A.alloc(8 * 36, BF16); wrlo = A.alloc(8 * 36, BF16)
    cp("dve", wrhi, wr, [bC], [bC])
    tt("dve", wrlo, wr, wrhi, ALU.subtract, [bC], [bC])
    wrhi3 = wrhi.rearrange("p (c n) -> p c n", c=8)
    wrlo3 = wrlo.rearrange("p (c n) -> p c n", c=8)
    bwin_h = P.buf("win_h")
    for c in range(8):
        dma("pool", win3[:, c, 0:1536], win_d[c * 128:(c + 1) * 128, 0:1536], writes=[bwin])
    for c in range(8):
        dma("pool", win3[:, c, 1536:3584], win_d[c * 128:(c + 1) * 128, 1536:3584], writes=[bwin_h])
    hT = A.alloc(8 * S, BF16); bhT = P.buf("hT")
    hT3 = hT.rearrange("p (c t) -> p c t", c=8)
    mixT = A.alloc(8 * S, BF16)
    mixT3 = mixT.rearrange("p (c t) -> p c t", c=8)
    bmix = [P.buf(f"mix{i}") for i in range(8)]
    scratch = A.top

    ident_bf, ident32 = C["ident_bf"], C["ident32"]
    ones_bf = C["ones_bf"]

    for sq in range(NSEQ):
        if sq > 0:
            P.barrier()
        A.top = scratch
        NX = 4
        xt = [A.alloc(D) for _ in range(NX)]; bxt = [P.buf() for _ in range(NX)]
        hb = [A.alloc(D, BF16) for _ in range(NX)]; bhb = [P.buf() for _ in range(NX)]
        sm1 = [A.alloc(2) for _ in range(NX)]; bsm1 = [P.buf() for _ in range(NX)]
        junk = A.alloc(D, BF16); bjunk = P.buf("junk")
        for t in range(NX - 1):
            dma("sp", xt[t], x_d[(sq * 16 + t) * 128:(sq * 16 + t + 1) * 128, :], writes=[bxt[t]])
        for t in range(16):
            gt = sq * 16 + t
            i = t % NX
            if t + NX - 1 < 16:
                t2 = t + NX - 1
                dma("sp", xt[t2 % NX], x_d[(sq * 16 + t2) * 128:(sq * 16 + t2 + 1) * 128, :], writes=[bxt[t2 % NX]])
            act(junk, xt[i], AF.Square, [bxt[i]], [bjunk, bsm1[i]], accum=sm1[i][:, 0:1])
            rsqrt_mean(sm1[i][:, 0:1], sm1[i][:, 0:1], D, [bsm1[i], bC], [bsm1[i]])
            stt("dve", hb[i], xt[i], sm1[i][:, 0:1], g1bc, ALU.mult, ALU.mult, [bxt[i], bsm1[i], bC], [bhb[i]])
            pb = rot("tr", [0, 1])
            for c in range(8):
                tr(PS(pb, BF16)[:, c * 128:(c + 1) * 128], hb[i][:, c * 128:(c + 1) * 128], ident_bf,
                   [bhb[i], bC], [pbuf[pb]])
            cp("act" if t % 2 else "dve", hT3[:, :, t * 128:(t + 1) * 128],
               PS(pb, BF16).rearrange("p (c t) -> p c t", c=8), [pbuf[pb]], [bhT])

        def proj_fm(col0, cb):
            pb = rot("proj", [0, 1])
            for c in range(8):
                mm(PS(pb), win3[:, c, col0:col0 + 128], hT3[:, c, cb * 512:(cb + 1) * 512], c == 0, c == 7,
                   [bwin if col0 < 1536 else bwin_h, bhT], [pbuf[pb]])
            return pb

        P.barrier()
        A.top = scratch
        QT = A.alloc(S, BF16); KT = A.alloc(S, BF16); bQT = P.buf(); bKT = P.buf()
        VcS = [[A.alloc(16 * 128, BF16) for _ in range(3)] for _ in range(2)]
        bVcS = [[P.buf() for _ in range(3)] for _ in range(2)]
        Uacc = A.alloc(S); Zacc = A.alloc(S); bUacc = P.buf(); bZacc = P.buf()
        ssacc = A.alloc(S); bss = P.buf()
        PT = [A.alloc(512, BF16) for _ in range(3)]; bPT = [P.buf() for _ in range(3)]
        osq = [A.alloc(512, BF16) for _ in range(2)]; bosq = [P.buf() for _ in range(2)]
        btab = A.alloc(12 * 128, BF16); bbt = P.buf()
        btab3 = btab.rearrange("p (j q) -> p j q", q=128)
        def vproj_hp(hp_):
            k_ = hp_ % 2
            Vn = VcS[k_][0]
            for g in range(4):
                pb = rot("proj", [0, 1])
                for j in range(4):
                    tl = g * 4 + j
                    for ch in range(8):
                        mm(PS(pb)[:, j * 128:(j + 1) * 128], hT3[:, ch, tl * 128:(tl + 1) * 128],
                           win3[:, ch, 1024 + hp_ * 128:1024 + (hp_ + 1) * 128], ch == 0, ch == 7,
                           [bwin, bhT], [pbuf[pb]])
                cp("act" if g % 2 else "dve", Vn[:, g * 512:(g + 1) * 512], PS(pb), [pbuf[pb]], [bVcS[k_][0]])
            dma("sp", vsd[k_].rearrange("(v p) f -> p v f", p=128), Vn.rearrange("p (v f) -> p v f", f=128),
                reads=[bVcS[k_][0]], writes=[b_vs[k_]])
            for r in range(4):
                dma("sp", VcS[k_][1].rearrange("p (r n f) -> p r n f", r=4, n=4)[:, r, :, :],
                    vsd[k_].rearrange("(n p r) f -> p r n f", n=4, p=128, r=4)[:, r, :, :],
                    reads=[b_vs[k_]], writes=[bVcS[k_][1]])
            dma("sp", VcS[k_][2].rearrange("p (r f) -> p r f", f=128), vsd[k_].rearrange("(p r) f -> p r f", r=16),
                reads=[b_vs[k_]], writes=[bVcS[k_][2]])

        vproj_hp(0)
        for hp in range(4):
            Vc, bVc = VcS[hp % 2], bVcS[hp % 2]
            dma("sp", btab, cd["btab"][:, hp * 1536:(hp + 1) * 1536], writes=[bbt])
            for cb in range(4):
                pb = proj_fm(hp * 128, cb)
                ts("dve", QT[:, cb * 512:(cb + 1) * 512], PS(pb), 0.125, None, ALU.mult, None, [pbuf[pb]], [bQT])
                pb = proj_fm(512 + hp * 128, cb)
                cp("act", KT[:, cb * 512:(cb + 1) * 512], PS(pb), [pbuf[pb]], [bKT])
            if hp + 1 < 4:
                vproj_hp(hp + 1)
            def att_scores(c, vb):
                pv = prev_vb(c, vb)
                kbs = [vb] + ([pv] if pv is not None else [])
                qs = tokslice(c, vb)
                pS = rot("S", [2, 3])
                ip = rot("PT", [0, 1, 2])
                for ty, kb in enumerate(kbs):
                    ks = tokslice(c, kb)
                    for e in range(2):
                        reg = PS(pS)[:, (ty * 2 + e) * 128:(ty * 2 + e + 1) * 128]
                        mm(reg, KT[64 * e:64 * e + 64, ks], QT[64 * e:64 * e + 64, qs], True, False,
                           [bKT, bQT], [pbuf[pS]])
                        mm(reg, ident_bf, btab3[:, (e * 3 + c) * 2 + ty, :], False, True, [bC, bbt], [pbuf[pS]])
                n = 256 * len(kbs)
                act(PT[ip][:, 0:n], PS(pS)[:, 0:n], AF.Exp, [pbuf[pS]], [bPT[ip]])
                return ip, kbs

            def att_pv(c, g, j, pU, pZ, ip, kbs):
                Vc3 = Vc[c].rearrange("p (v f) -> p v f", f=128)
                for e in range(2):
                    for ty, kb in enumerate(kbs):
                        mm(PS(pU)[64 * e:64 * e + 64, j * 128:(j + 1) * 128], Vc3[:, kb, 64 * e:64 * e + 64],
                           PT[ip][:, (ty * 2 + e) * 128:(ty * 2 + e + 1) * 128], ty == 0, ty == len(kbs) - 1,
                           [bVc[c], bPT[ip]], [pbuf[pU]])
                    for ty, kb in enumerate(kbs):
                        mm(PS(pZ)[64 * e:64 * e + 64, j * 128:(j + 1) * 128], ones_bf[:, 0:64],
                           PT[ip][:, (ty * 2 + e) * 128:(ty * 2 + e + 1) * 128], ty == 0, ty == len(kbs) - 1,
                           [bC, bPT[ip]], [pbuf[pZ]])
                if j == 3:
                    for (acc, bacc, pb_) in ((Uacc, bUacc, pU), (Zacc, bZacc, pZ)):
                        if c == 0:
                            dst = acc[:, g * 512:(g + 1) * 512]
                            src = PS(pb_)
                        elif c == 1:
                            dst = acc[:, g:g + 4 * 511 + 1:4]
                            src = PS(pb_)
                        else:
                            dst = acc.rearrange("a (p r) -> a r p", r=16)[:, 4 * g:4 * g + 4, :]
                            src = PS(pb_).rearrange("a (j p) -> a j p", j=4)
                        if c == 0:
                            cp("dve", dst, src, [pbuf[pb_]], [bacc])
                        else:
                            tt("dve", dst, dst, src, ALU.add, [pbuf[pb_], bacc], [bacc])

            pend = None
            for c in range(3):
                for g in range(4):
                    pU = rot("U", [4, 5])
                    pZ = rot("Z", [6, 7])
                    for j in range(4):
                        ip, kbs = att_scores(c, g * 4 + j)
                        if pend is not None:
                            att_pv(*pend)
                        pend = (c, g, j, pU, pZ, ip, kbs)
            att_pv(*pend)
            act(Zacc, Zacc, AF.Ln, [bZacc], [bZacc])
            act(Zacc, Zacc, AF.Exp, [bZacc], [bZacc], scale=-1.0)
            tt("dve", Uacc, Uacc, Zacc, ALU.mult, [bUacc, bZacc], [bUacc])
            cp("pool", mixT3[:, hp, :], Uacc, [bUacc], [bmix[hp]])
            for cb in range(4):
                io = rot("osq", [0, 1])
                act(osq[io], Uacc[:, cb * 512:(cb + 1) * 512], AF.Square, [bUacc], [bosq[io]])
                pb = rot("proj", [0, 1])
                mm(PS(pb), ones_bf, osq[io], True, True, [bC, bosq[io]], [pbuf[pb]])
                if hp == 0:
                    cp("dve", ssacc[:, cb * 512:(cb + 1) * 512], PS(pb), [pbuf[pb]], [bss])
                else:
                    tt("dve", ssacc[:, cb * 512:(cb + 1) * 512], ssacc[:, cb * 512:(cb + 1) * 512], PS(pb), ALU.add,
                       [pbuf[pb], bss], [bss])
        rsqrt_mean(ssacc, ssacc, 512, [bss, bC], [bss])
        for hp in range(4):
            stt("dve", mixT3[:, hp, :], mixT3[:, hp, :], ga[:, hp:hp + 1], ssacc, ALU.mult, ALU.mult,
                [bmix[hp], bss, bC], [bmix[hp]])

        P.barrier()
        A.top = scratch
        qinT = A.alloc(S, BF16); kinT = A.alloc(S, BF16); kendT = A.alloc(S, BF16); gsT = A.alloc(S, BF16)
        bqin = P.buf(); bkin = P.buf(); bkend = P.buf(); bgs = P.buf()
        iV = A.alloc(16 * 128, BF16); biV = P.buf()
        dec = A.alloc(16); bdec = P.buf()
        sigf = A.alloc(S); bsigf = [P.buf() for _ in range(2)]
        tBf = A.alloc(S); btB = [P.buf() for _ in range(2)]
        tEf = A.alloc(S); btE = [P.buf() for _ in range(2)]
        tCf = A.alloc(S); btC = [P.buf() for _ in range(2)]
        qsT = A.alloc(S, BF16); bqs = P.buf()
        Am = [A.alloc(256, BF16) for _ in range(2)]; bAmh = [[P.buf(), P.buf()] for _ in range(2)]
        ket = [A.alloc(512, BF16) for _ in range(2)]; bket = [P.buf() for _ in range(2)]
        osq = [A.alloc(512, BF16) for _ in range(2)]; bosq = [P.buf() for _ in range(2)]
        decm = A.alloc(1024); bdecm = P.buf()
        Sall = A.alloc(1024, BF16); bSall = P.buf()
        Sall3 = Sall.rearrange("p (v t) -> p v t", t=16)
        if sq == 0:
            nr = (NSLOT + 128) // 4
            for i in range(4):
                dma("act", xg[i * nr:(i + 1) * nr, :], cd["zeros_bf"][i * nr:(i + 1) * nr, :], writes=[b_xgz[i]])
            dma("act", ybuf[NSLOT:NSLOT + 128, :], cd["zeros_bf"][0:128, :], writes=[b_ybuf])
        for hp in range(4):
            cq, cf, ci, cg = 1536 + hp * 128, 2048 + hp * 128, 2560 + hp * 128, 3072 + hp * 128
            for cb in range(4):
                cs = slice(cb * 512, (cb + 1) * 512)
                pf = proj_fm(cf, cb)
                act(sigf[:, cs], PS(pf), AF.Sigmoid, [pbuf[pf]], [bsigf[cb // 2]])
            for cb in range(4):
                cs = slice(cb * 512, (cb + 1) * 512)
                pq = proj_fm(cq, cb)
                act(qsT[:, cs], PS(pq), AF.Silu, [pbuf[pq]], [bqs])
                pg = proj_fm(cg, cb)
                act(gsT[:, cs], PS(pg), AF.Silu, [pbuf[pg]], [bgs])
            HS = [slice(0, 1024), slice(1024, 2048)]
            for h in range(2):
                ts("dve", tBf[:, HS[h]], sigf[:, HS[h]], oml[:, hp:hp + 1], lb[:, hp:hp + 1], ALU.mult, ALU.add,
                   [bsigf[h], bC], [btB[h]])
            for h in range(2):
                act(tBf[:, HS[h]], tBf[:, HS[h]], AF.Ln, [btB[h]], [btB[h]])
            for h in range(2):
                ts("dve", sigf[:, HS[h]], sigf[:, HS[h]], noml[:, hp:hp + 1], oml[:, hp:hp + 1], ALU.mult, ALU.add,
                   [bsigf[h], bC], [bsigf[h]])
            for h in range(2):
                for j in range(2):
                    c5 = slice(h * 1024 + j * 512, h * 1024 + (j + 1) * 512)
                    P.op("dve", lambda e, o_=tEf[:, c5], d_=tBf[:, c5], m_=C["scanmask"]: e.tensor_tensor_scan(
                        out=o_, data0=m_, data1=d_, initial=0.0, op0=ALU.mult, op1=ALU.add), [btB[h], bC], [btE[h]])
            for h in range(2):
                act(tCf[:, HS[h]], tEf[:, HS[h]], AF.Exp, [btE[h]], [btC[h]])
            for h in range(2):
                tt("dve", qinT[:, HS[h]], qsT[:, HS[h]], tCf[:, HS[h]], ALU.mult, [bqs, btC[h]], [bqin])
            for h in range(2):
                act(tCf[:, HS[h]], tEf[:, HS[h]], AF.Exp, [btE[h], btC[h]], [btC[h]], scale=-1.0)
            act(dec[:, 0:16], tEf[:, 127:2048:128], AF.Exp, [btE[0], btE[1]], [bdec])
            for h in range(2):
                tt("dve", kinT[:, HS[h]], sigf[:, HS[h]], tCf[:, HS[h]], ALU.mult, [bsigf[h], btC[h]], [bkin])
            for h in range(2):
                tt("dve", kendT[:, HS[h]].rearrange("p (a b) -> p a b", b=128), kinT[:, HS[h]].rearrange("p (a b) -> p a b", b=128),
                   dec[:, h * 8:(h + 1) * 8].unsqueeze(2).to_broadcast([128, 8, 128]), ALU.mult, [bkin, bdec], [bkend])
            for g in range(4):
                pb = g
                for j in range(4):
                    tl = g * 4 + j
                    for ch in range(8):
                        mm(PS(pb)[:, j * 128:(j + 1) * 128], hT3[:, ch, tl * 128:(tl + 1) * 128], win3[:, ch, ci:ci + 128],
                           ch == 0, ch == 7, [bwin_h, bhT], [pbuf[pb]])
                cp("act" if g % 2 else "dve", iV[:, g * 512:(g + 1) * 512], PS(pb), [pbuf[pb]], [biV])
            iV3 = iV.rearrange("p (v f) -> p v f", f=128)
            cp("pool", decm.rearrange("p (v t) -> p v t", t=16), dec[:, 0:16].unsqueeze(1).to_broadcast([128, 64, 16]),
               [bdec], [bdecm])
            P.op("pool", lambda e: e.memset(decm[:, 0:1024:16], 0.0), [bdecm], [bdecm])
            for g4 in range(4):
                i = g4 % 2
                pt_ = 4 + i
                for j in range(4):
                    tk = g4 * 4 + j
                    tr(PS(pt_, BF16)[:, j * 128:(j + 1) * 128], kendT[:, tk * 128:(tk + 1) * 128], ident_bf, [bkend, bC], [pbuf[pt_]])
                cp("act" if i else "dve", ket[i], PS(pt_, BF16)[:, 0:512], [pbuf[pt_]], [bket[i]])
                for j in range(4):
                    tk = g4 * 4 + j
                    for e in range(2):
                        for vh in range(2):
                            mm(PS(vh)[64 * e:64 * e + 64, tk:512:16], ket[i][:, j * 128 + 64 * e:j * 128 + 64 * e + 64],
                               iV3[:, tk, 64 * e + 32 * vh:64 * e + 32 * vh + 32], True, True, [bket[i], biV], [pbuf[vh]])
            for vh in range(2):
                P.op("dve", lambda e, o_=Sall[:, vh * 512:(vh + 1) * 512], d0=decm[:, vh * 512:(vh + 1) * 512], d1=PS(vh):
                     e.tensor_tensor_scan(out=o_, data0=d0, data1=d1, initial=0.0, op0=ALU.mult, op1=ALU.add),
                     [bdecm, pbuf[vh]], [bSall])

            def stage1(tk):
                i = tk % 2
                cs = slice(tk * 128, (tk + 1) * 128)
                ab_ = 2 if tk % 2 == 0 else 0
                for e in range(2):
                    mm(PS(ab_ + e)[:, 0:128], kinT[64 * e:64 * e + 64, cs], qinT[64 * e:64 * e + 64, cs], True, True,
                       [bkin, bqin], [pbuf[ab_ + e]])
                for e in range(2):
                    tt("dve", Am[i][:, e * 128:(e + 1) * 128], PS(ab_ + e)[:, 0:128], C["cmask"][:, 0:128], ALU.mult,
                       [pbuf[ab_ + e], bC], [bAmh[i][e]])

            def stage2(tk, po):
                i = tk % 2
                cs = slice(tk * 128, (tk + 1) * 128)
                j = tk % 4
                for e in range(2):
                    mm(PS(po)[64 * e:64 * e + 64, j * 128:(j + 1) * 128], iV3[:, tk, 64 * e:64 * e + 64],
                       Am[i][:, e * 128:(e + 1) * 128], True, tk == 0, [biV, bAmh[i][e]], [pbuf[po]])
                    if tk > 0:
                        mm(PS(po)[64 * e:64 * e + 64, j * 128:(j + 1) * 128], Sall3[64 * e:64 * e + 64, :, tk - 1],
                           qinT[64 * e:64 * e + 64, cs], False, True, [bSall, bqin], [pbuf[po]])

            def post_a(cb, po):
                io = cb % 2
                act(osq[io], PS(po), AF.Square, [pbuf[po]], [bosq[io]])

            def post_b(cb, po):
                io = cb % 2
                tmp = tBf[:, cb * 512:(cb + 1) * 512]
                mm(PS(5), C["bd_bf"], osq[io], True, True, [bC, bosq[io]], [pbuf[5]])
                rsqrt_mean(tmp, PS(5), 64, [pbuf[5], bC], [btB[cb // 2]])

            def post_c(cb, po):
                cs = slice(cb * 512, (cb + 1) * 512)
                tmp = tBf[:, cs]
                stt("dve", tmp, PS(po), gh[:, hp:hp + 1], tmp, ALU.mult, ALU.mult, [pbuf[po], bC, btB[cb // 2]], [btB[cb // 2]])
                tt("pool", mixT3[:, 4 + hp, cs], tmp, gsT[:, cs], ALU.mult, [btB[cb // 2], bgs], [bmix[4 + hp]])

            po = None
            pos_ = {}
            stage1(0)
            for tk in range(16):
                if tk + 1 < 16:
                    stage1(tk + 1)
                if tk % 4 == 0:
                    po = rot("O", [6, 7])
                    pos_[tk // 4] = po
                stage2(tk, po)
                g_ = tk // 4
                if tk % 4 == 3:
                    post_a(g_, po)
                if tk % 4 == 0 and g_ >= 1:
                    post_b(g_ - 1, pos_[g_ - 1])
                if tk % 4 == 2 and g_ >= 1:
                    post_c(g_ - 1, pos_[g_ - 1])
            post_b(3, pos_[3])
            post_c(3, pos_[3])

        P.barrier()
        A.top = scratch
        wout = A.alloc(8 * D, BF16); bwout = P.buf("wout")
        wout3 = wout.rearrange("p (c n) -> p c n", c=8)
        dma("pool", wout3, wout_d.rearrange("(c p) n -> p c n", p=128), writes=[bwout])
        xt = [A.alloc(D) for _ in range(2)]; bxt = [P.buf() for _ in range(2)]
        sm1 = [A.alloc(2) for _ in range(2)]; bsm1 = [P.buf() for _ in range(2)]
        junk = A.alloc(D, BF16); bjunk = P.buf("junk")
        x1t = [A.alloc(D) for _ in range(2)]; bx1 = [P.buf() for _ in range(2)]
        h2f = [A.alloc(D) for _ in range(2)]; bh2f = [P.buf() for _ in range(2)]
        h2b = [A.alloc(D, BF16) for _ in range(2)]; bh2b = [P.buf() for _ in range(2)]
        h2lo = [A.alloc(D, BF16) for _ in range(2)]; bh2lo = [P.buf() for _ in range(2)]
        hiT = [A.alloc(D, BF16) for _ in range(2)]; bhiT = [P.buf() for _ in range(2)]
        loT = [A.alloc(D, BF16) for _ in range(2)]; bloT = [P.buf() for _ in range(2)]
        def op_a(t):
            gt = sq * 16 + t
            i = gt % 2
            pa, pb2 = (0, 1) if i == 0 else (2, 3)
            dma("sp", xt[i], x_d[gt * 128:(gt + 1) * 128, :], writes=[bxt[i]])
            for half, pb in ((0, pa), (1, pb2)):
                for c in range(8):
                    mm(PS(pb), mixT3[:, c, t * 128:(t + 1) * 128], wout3[:, c, half * 512:(half + 1) * 512], c == 0, c == 7,
                       [bmix[c], bwout], [pbuf[pb]])
                tt("dve", x1t[i][:, half * 512:(half + 1) * 512], xt[i][:, half * 512:(half + 1) * 512], PS(pb), ALU.add,
                   [bxt[i], pbuf[pb]], [bx1[i]])
            dma("sp", x1s[gt * 128:(gt + 1) * 128, :], x1t[i], reads=[bx1[i]], writes=[b_x1s[gt]])
            act(junk, x1t[i], AF.Square, [bx1[i]], [bjunk, bsm1[i]], accum=sm1[i][:, 0:1])
            rsqrt_mean(sm1[i][:, 0:1], sm1[i][:, 0:1], D, [bsm1[i], bC], [bsm1[i]])
            stt("dve", h2f[i], x1t[i], sm1[i][:, 0:1], g2bc, ALU.mult, ALU.mult, [bx1[i], bsm1[i], bC], [bh2f[i]])
            cp("pool", h2b[i], h2f[i], [bh2f[i]], [bh2b[i]])
            dma("sp", h2s[gt * 128:(gt + 1) * 128, :], h2b[i], reads=[bh2b[i]], writes=[b_h2s[gt]])

        def op_b(t):
            gt = sq * 16 + t
            i = gt % 2
            tt("pool", h2lo[i], h2f[i], h2b[i], ALU.subtract, [bh2f[i], bh2b[i]], [bh2lo[i]])
            for (src, bsrc, pbt, dstT, bdst, eng) in ((h2b, bh2b, 4, hiT, bhiT, "act"), (h2lo, bh2lo, 5, loT, bloT, "dve")):
                for c in range(8):
                    tr(PS(pbt, BF16)[:, c * 128:(c + 1) * 128], src[i][:, c * 128:(c + 1) * 128], ident_bf, [bsrc[i], bC],
                       [pbuf[pbt]])
                cp(eng, dstT[i], PS(pbt, BF16)[:, 0:1024], [pbuf[pbt]], [bdst[i]])
            pl = rot("L", [6, 7])
            k_ = 0
            for c in range(8):
                for (aT, baT, w3) in ((hiT, bhiT, wrhi3), (loT, bloT, wrhi3), (hiT, bhiT, wrlo3)):
                    mm(PS(pl)[:, 0:36], aT[i][:, c * 128:(c + 1) * 128], w3[:, c, :], k_ == 0, k_ == 23, [baT[i], bC], [pbuf[pl]])
                    k_ += 1
            tt("dve", Lall[:, gt * 36:(gt + 1) * 36], PS(pl)[:, 0:36], brbc, ALU.add, [pbuf[pl], bC], [bL])

        op_a(0)
        for t in range(16):
            if t + 1 < 16:
                op_a(t + 1)
            op_b(t)

    P.barrier()
    A.top = top_persist
    L3 = Lall.rearrange("p (t n) -> p t n", n=36)

    def al(n, dt=F32):
        return A.alloc(n, dt)
    bR = P.buf("route")
    gate1 = al(NT); gate2 = al(NT); d1i = al(NT, I32); d2i = al(NT, I32)
    top_route = A.top
    gmax = al(NT); goh = al(NT * 4); gexp = al(NT * 4); gsum = al(NT); wgt = al(NT)
    sel = al(NT * 8); tmp8 = al(NT * 8); m1 = al(NT); m2 = al(NT); oh1 = al(NT * 8); oh2 = al(NT * 8)
    dd = al(NT)
    A1 = al(NT * 32); A2 = al(NT * 32); Aall = al(NT * 32, BF16); pos = al(NT * 32); tot = al(NT * 32)
    cum = al(NT * 32); tmp32 = al(NT * 32); valid = al(NT * 32)
    d1f = al(NT); d2f = al(NT)
    RW = [bL, bR, bC]

    def v3(ap, n):
        return ap.rearrange("p (t n) -> p t n", n=n)

    def bc(ap, n):
        return ap.unsqueeze(2).to_broadcast([128, NT, n])

    P.op("dve", lambda e: e.tensor_reduce(out=gmax, in_=L3[:, :, 0:4], axis=AX.X, op=ALU.max), RW, RW)
    tt("dve", v3(goh, 4), L3[:, :, 0:4], bc(gmax, 4), ALU.is_equal, RW, RW)
    tt("dve", v3(gexp, 4), L3[:, :, 0:4], bc(gmax, 4), ALU.subtract, RW, RW)
    act(gexp, gexp, AF.Exp, RW, RW)
    P.op("dve", lambda e: e.tensor_reduce(out=gsum, in_=v3(gexp, 4), axis=AX.X, op=ALU.add), RW, RW)
    P.op("dve", lambda e: e.reciprocal(out=wgt, in_=gsum), RW, RW)
    for g in range(4):
        src = L3[:, :, 4 + 8 * g:12 + 8 * g]
        gsel = v3(goh, 4)[:, :, g:g + 1].to_broadcast([128, NT, 8])
        if g == 0:
            tt("dve", v3(sel, 8), src, gsel, ALU.mult, RW, RW)
        else:
            tt("dve", v3(tmp8, 8), src, gsel, ALU.mult, RW, RW)
            tt("dve", sel, sel, tmp8, ALU.add, RW, RW)
    P.op("dve", lambda e: e.tensor_reduce(out=m1, in_=v3(sel, 8), axis=AX.X, op=ALU.max), RW, RW)
    tt("dve", v3(oh1, 8), v3(sel, 8), bc(m1, 8), ALU.is_equal, RW, RW)
    stt("dve", tmp8, oh1, -1e30, sel, ALU.mult, ALU.add, RW, RW)
    P.op("dve", lambda e: e.tensor_reduce(out=m2, in_=v3(tmp8, 8), axis=AX.X, op=ALU.max), RW, RW)
    tt("dve", v3(oh2, 8), v3(tmp8, 8), bc(m2, 8), ALU.is_equal, RW, RW)
    tt("dve", dd, m2, m1, ALU.subtract, RW, RW)
    act(dd, dd, AF.Exp, RW, RW)
    ts("dve", dd, dd, 1.0, None, ALU.add, None, RW, RW)
    P.op("dve", lambda e: e.reciprocal(out=dd, in_=dd), RW, RW)
    tt("dve", gate1, wgt, dd, ALU.mult, RW, RW)
    tt("dve", gate2, wgt, gate1, ALU.subtract, RW, RW)
    for (Ak, ohk) in ((A1, oh1), (A2, oh2)):
        tt("dve", Ak.rearrange("p (t g j) -> p t g j", g=4, j=8),
           v3(goh, 4).unsqueeze(3).to_broadcast([128, NT, 4, 8]),
           v3(ohk, 8).unsqueeze(2).to_broadcast([128, NT, 4, 8]), ALU.mult, RW, RW)
    tt("dve", Aall, A1, A2, ALU.add, RW, RW)
    for hf in range(2):
        sl = slice(hf * 512, (hf + 1) * 512)
        mm(PS(hf), C["tri_bf"], Aall[:, sl], True, True, RW, [pbuf[hf]])
        mm(PS(2 + hf), ones_bf, Aall[:, sl], True, True, RW, [pbuf[2 + hf]])
        cp("dve", pos[:, sl], PS(hf), [pbuf[hf]], RW)
        cp("dve", tot[:, sl], PS(2 + hf), [pbuf[2 + hf]], RW)
    P.op("dve", lambda e: e.memset(cum[:, 0:32], 0.0), RW, RW)
    for t in range(1, NT):
        tt("dve", cum[:, t * 32:(t + 1) * 32], cum[:, (t - 1) * 32:t * 32], tot[:, (t - 1) * 32:t * 32], ALU.add, RW, RW)
    tt("dve", pos, pos, cum, ALU.add, RW, RW)
    ts("dve", valid, pos, float(CAP), None, ALU.is_lt, None, RW, RW)
    tt("dve", v3(pos, 32), v3(pos, 32), C["ebase"].unsqueeze(1).to_broadcast([128, NT, 32]), ALU.add, RW, RW)
    ts("dve", pos, pos, float(-TRASH), None, ALU.add, None, RW, RW)
    tt("dve", pos, pos, valid, ALU.mult, RW, RW)
    for (Ak, dkf, dki) in ((A1, d1f, d1i), (A2, d2f, d2i)):
        tt("dve", tmp32, pos, Ak, ALU.mult, RW, RW)
        P.op("dve", lambda e, o_=dkf: e.tensor_reduce(out=o_, in_=v3(tmp32, 32), axis=AX.X, op=ALU.add), RW, RW)
        ts("dve", dkf, dkf, float(TRASH), None, ALU.add, None, RW, RW)
        cp("dve", dki, dkf, RW, RW)

    P.barrier()
    A.top = top_route
    NH = 6
    b_sc = [P.buf() for _ in range(2 * NT)]
    b_sc_used = []
    _hs_top = A.top
    hsb = [A.alloc(D, BF16) for _ in range(NH)]; bhsb = [P.buf() for _ in range(NH)]
    for gt in range(NT):
        i = gt % NH
        dma("sp", hsb[i], h2s[gt * 128:(gt + 1) * 128, :], reads=[b_h2s[gt]], writes=[bhsb[i]])
        for dki in (d1i, d2i):
            P.op("pool", lambda e, s_=hsb[i], o_=dki[:, gt:gt + 1]: e.indirect_dma_start(
                out=xg, out_offset=bass.IndirectOffsetOnAxis(ap=o_, axis=0), in_=s_, in_offset=None),
                [bhsb[i], bR] + b_xgz, [b_sc[len(b_sc_used)]], dma=True)
            b_sc_used.append(1)

    A.top = _hs_top
    XT = [A.alloc(8 * CAP, BF16) for _ in range(2)]; bXT = [P.buf() for _ in range(2)]
    assert A.top >= _hs_top + NH * (D // 2)
    wgb = [A.alloc(8 * 512, BF16) for _ in range(2)]; bwg = [P.buf() for _ in range(2)]
    wub = [A.alloc(8 * 512, BF16) for _ in range(2)]; bwu = [P.buf() for _ in range(2)]
    wdb = [A.alloc(4 * D, BF16) for _ in range(2)]; bwd = [P.buf() for _ in range(2)]
    stage = [[A.alloc(4096) for _ in range(3)] for _ in range(2)]; bstg = [[P.buf() for _ in range(3)] for _ in range(2)]
    NST = CAP // 128
    xgt = [A.alloc(NST * D, BF16) for _ in range(2)]; bxgt = [P.buf() for _ in range(2)]
    sg = [A.alloc(CAP) for _ in range(2)]; bsg = [P.buf() for _ in range(2)]
    hidT = [A.alloc(4 * CAP, BF16) for _ in range(2)]; bhid = [P.buf() for _ in range(2)]
    yt = [A.alloc(D, BF16) for _ in range(3)]; byt = [P.buf() for _ in range(3)]

    b_ys = []

    def loads(ex):
        k = ex % 2
        dma("sp", stage[k][0].rearrange("p (c n) -> p c n", c=8), wg_d[ex].rearrange("(c p) n -> p c n", p=128), writes=[bstg[k][0]])
        dma("sp", stage[k][1].rearrange("p (c n) -> p c n", c=8), wu_d[ex].rearrange("(c p) n -> p c n", p=128), writes=[bstg[k][1]])
        dma("sp", stage[k][2].rearrange("p (c n) -> p c n", c=4), wd_d[ex].rearrange("(c p) n -> p c n", p=128), writes=[bstg[k][2]])

    def load_x(ex):
        i = ex % 2
        dma("sp", xgt[i].rearrange("p (s d) -> p s d", s=NST), xg[ex * CAP:(ex + 1) * CAP, :].rearrange("(s p) d -> p s d", p=128),
            reads=b_sc, writes=[bxgt[i]])

    def casts_gu(ex):
        i = ex % 2
        cp("act", wgb[i][:, 0:2048], stage[i][0][:, 0:2048], [bstg[i][0]], [bwg[i]])
        cp("dve", wgb[i][:, 2048:4096], stage[i][0][:, 2048:4096], [bstg[i][0]], [bwg[i]])
        cp("dve", wub[i][:, 0:2048], stage[i][1][:, 0:2048], [bstg[i][1]], [bwu[i]])
        cp("act", wub[i][:, 2048:4096], stage[i][1][:, 2048:4096], [bstg[i][1]], [bwu[i]])

    def cast_d(ex):
        i = ex % 2
        cp("pool", wdb[i][:, 0:2560], stage[i][2][:, 0:2560], [bstg[i][2]], [bwd[i]])
        cp("act", wdb[i][:, 2560:3328], stage[i][2][:, 2560:3328], [bstg[i][2]], [bwd[i]])
        cp("dve", wdb[i][:, 3328:4096], stage[i][2][:, 3328:4096], [bstg[i][2]], [bwd[i]])

    loads(0); load_x(0); loads(1); load_x(1); casts_gu(0); cast_d(0)
    for ex in range(NE):
        i = ex % 2
        XT3 = XT[i].rearrange("p (c s) -> p c s", c=8)
        for s4 in range(NST):
            pb = rot("xtr", [0, 1])
            for c in range(8):
                tr(PS(pb, BF16)[:, c * 128:(c + 1) * 128], xgt[i][:, s4 * D + c * 128:s4 * D + (c + 1) * 128], ident_bf,
                   [bxgt[i], bC], [pbuf[pb]])
            cp("act" if s4 % 2 else "dve", XT3[:, :, s4 * 128:(s4 + 1) * 128], PS(pb, BF16).rearrange("p (c t) -> p c t", c=8),
               [pbuf[pb]], [bXT[i]])
        if ex + 2 < NE:
            load_x(ex + 2)
        wg3 = wgb[i].rearrange("p (c n) -> p c n", c=8)
        wu3 = wub[i].rearrange("p (c n) -> p c n", c=8)
        wd3 = wdb[i].rearrange("p (c n) -> p c n", c=4)
        for fc in range(4):
            pG = rot("G", [2, 3])
            pUp = rot("Up", [4, 5])
            for c in range(8):
                mm(PS(pG)[:, 0:CAP], wg3[:, c, fc * 128:(fc + 1) * 128], XT3[:, c, :], c == 0, c == 7, [bwg[i], bXT[i]], [pbuf[pG]])
            for c in range(8):
                mm(PS(pUp)[:, 0:CAP], wu3[:, c, fc * 128:(fc + 1) * 128], XT3[:, c, :], c == 0, c == 7, [bwu[i], bXT[i]], [pbuf[pUp]])
            isg = rot("sg", [0, 1])
            act(sg[isg], PS(pG)[:, 0:CAP], AF.Silu, [pbuf[pG]], [bsg[isg]])
            tt("dve", hidT[i][:, fc * CAP:(fc + 1) * CAP], sg[isg], PS(pUp)[:, 0:CAP], ALU.mult, [bsg[isg], pbuf[pUp]], [bhid[i]])
        for s4 in range(NST):
            iy = rot("yt", [0, 1, 2])
            for half in range(2):
                pY = rot("Y", [6, 7])
                for fc in range(4):
                    mm(PS(pY), hidT[i][:, fc * CAP + s4 * 128:fc * CAP + (s4 + 1) * 128], wd3[:, fc, half * 512:(half + 1) * 512],
                       fc == 0, fc == 3, [bhid[i], bwd[i]], [pbuf[pY]])
                cp("act" if half else "dve", yt[iy][:, half * 512:(half + 1) * 512], PS(pY),
                   [pbuf[pY]], [byt[iy]])
            r0 = ex * CAP + s4 * 128
            b_ys.append(P.buf())
            dma("sp", ybuf[r0:r0 + 128, :], yt[iy], reads=[byt[iy]], writes=[b_ys[-1]])
        if ex + 1 < NE:
            cast_d(ex + 1)
            casts_gu(ex + 1)
        if ex + 2 < NE:
            loads(ex + 2)

    P.barrier()
    A.top = top_route
    NB = 4
    gfbc = A.alloc(D); bgf = P.buf()
    dma("sp", gfbc, gf_d, writes=[bgf])
    x1c = [A.alloc(D) for _ in range(NB)]; bx1c = [P.buf() for _ in range(NB)]
    Y1 = [A.alloc(D, BF16) for _ in range(NB)]; bY1 = [P.buf() for _ in range(NB)]
    Y2 = [A.alloc(D, BF16) for _ in range(NB)]; bY2 = [P.buf() for _ in range(NB)]
    oo = [A.alloc(D) for _ in range(NB)]; boo = [P.buf() for _ in range(NB)]
    jk2 = A.alloc(D, BF16); bjk2 = P.buf()
    smf = [A.alloc(2) for _ in range(NB)]; bsmf = [P.buf() for _ in range(NB)]

    def cload(gt):
        i = gt % NB
        dma("sp", x1c[i], x1s[gt * 128:(gt + 1) * 128, :], reads=[b_x1s[gt]], writes=[bx1c[i]])
        for (Yk, bYk, dki) in ((Y1, bY1, d1i), (Y2, bY2, d2i)):
            P.op("pool", lambda e, o_=Yk[i], x_=dki[:, gt:gt + 1]: e.indirect_dma_start(
                out=o_, out_offset=None, in_=ybuf, in_offset=bass.IndirectOffsetOnAxis(ap=x_, axis=0)),
                [b_ybuf, bR] + b_ys, [bYk[i]], dma=True)

    for gt in range(NB - 1):
        cload(gt)
    for gt in range(NT):
        i = gt % NB
        if gt + NB - 1 < NT:
            cload(gt + NB - 1)
        stt("dve", x1c[i], Y1[i], gate1[:, gt:gt + 1], x1c[i], ALU.mult, ALU.add, [bY1[i], bR, bx1c[i]], [bx1c[i]])
        stt("dve", x1c[i], Y2[i], gate2[:, gt:gt + 1], x1c[i], ALU.mult, ALU.add, [bY2[i], bR, bx1c[i]], [bx1c[i]])
        act(jk2, x1c[i], AF.Square, [bx1c[i]], [bjk2, bsmf[i]], accum=smf[i][:, 0:1])
        rsqrt_mean(smf[i][:, 0:1], smf[i][:, 0:1], D, [bsmf[i], bC], [bsmf[i]])
        act(oo[i], x1c[i], AF.Copy, [bx1c[i], bsmf[i]], [boo[i]], scale=smf[i][:, 0:1])
        tt("pool", oo[i], oo[i], gfbc, ALU.mult, [boo[i], bgf], [boo[i]])
        dma("sp", out_d[gt * 128:(gt + 1) * 128, :], oo[i], reads=[boo[i]], writes=[b_out])
    P.final_wait("sp")
    P.emit(st)
    st.close()
    return nc, P


_CACHE = {}


def kernel(x, norm1_g, w_in, attn_norm_g, hgrn_gamma, hgrn_norm_g, w_out, norm2_g,
           w_group, b_group, w_router, b_router, w_gate, w_up, w_down, norm_f_g):
    f32 = np.float32
    x = np.asarray(x, f32)
    if "nc" not in _CACHE:
        _CACHE["nc"] = build()
    nc, _ = _CACHE["nc"]
    cons = host_consts()

    def bc128(v):
        return np.ascontiguousarray(np.broadcast_to(np.asarray(v, f32).reshape(1, -1), (128, v.size)))

    def fm(v, c):
        return np.ascontiguousarray(np.asarray(v, f32).reshape(c, 128).T)

    wr = np.concatenate([np.asarray(w_group, f32)[0], np.asarray(w_router, f32)[0]], axis=1)
    br = np.concatenate([np.asarray(b_group, f32)[0], np.asarray(b_router, f32)[0]], axis=0)
    shared = {
        "w_in": np.ascontiguousarray(np.asarray(w_in, f32)[0]),
        "w_out": np.ascontiguousarray(np.asarray(w_out, f32)[0]),
        "g1bc": bc128(np.asarray(norm1_g, f32)[0]),
        "g2bc": bc128(np.asarray(norm2_g, f32)[0]),
        "gfbc": bc128(np.asarray(norm_f_g, f32)),
        "ga": fm(np.asarray(attn_norm_g, f32)[0], 4),
        "gh": fm(np.asarray(hgrn_norm_g, f32)[0], 4),
        "gam": np.ascontiguousarray(np.concatenate([fm(np.asarray(hgrn_gamma, f32)[0], 4),
                                                    fm(np.asarray(hgrn_gamma, f32)[1], 4)], axis=1)),
        "wr": np.ascontiguousarray(wr.reshape(8, 128, 36).transpose(1, 0, 2)),
        "brbc": bc128(br),
        "w_gate": np.ascontiguousarray(np.asarray(w_gate, f32)[0]),
        "w_up": np.ascontiguousarray(np.asarray(w_up, f32)[0]),
        "w_down": np.ascontiguousarray(np.asarray(w_down, f32)[0]),
    }
    for k, v in cons.items():
        shared["c_" + k] = v
    xs = x.reshape(NCORES, T, D)
    in_maps = [dict(shared, x=np.ascontiguousarray(xs[i])) for i in range(NCORES)]
    res = run_bass_kernel_spmd(nc, in_maps, core_ids=list(range(NCORES)))
    out = np.stack([np.asarray(r["out"], f32).reshape(T, D) for r in res.results], axis=0)
    return out.reshape(16, S, D)
```

```python
import numpy as np
import ml_dtypes
from contextlib import ExitStack
import concourse.bass as bass
import concourse.mybir as mybir
from concourse.bass_utils import run_bass_kernel_spmd

F32 = mybir.dt.float32
BF16 = mybir.dt.bfloat16
I32 = mybir.dt.int32
AF = mybir.ActivationFunctionType
ALU = mybir.AluOpType
AX = mybir.AxisListType

COMPUTE = ("pe", "act", "dve", "pool")
QUEUES = ("sp", "act", "pool")
KSLOTS = {"sp": 8, "act": 8, "pool": 8}
ENGS = ("pe", "act", "dve", "pool", "sp")


class Buf:
    __slots__ = ("name", "lw", "rd")

    def __init__(self, name):
        self.name = name
        self.lw = None
        self.rd = []


class Op:
    __slots__ = ("eng", "fn", "tl", "ord", "waits", "clock", "is_dma", "idx")


class Prog:
    def __init__(self, nc, same_engine_sync=("act", "dve", "pool")):
        self.nc = nc
        self.ops = []
        self.eng_ops = {e: [] for e in ENGS}
        self.known = {e: {} for e in ENGS}
        self.cnt = {e: 0 for e in COMPUTE}
        self.dcnt = {q: 0 for q in QUEUES}
        self.last = {}
        self.same_sync = set(same_engine_sync)
        self.nbuf = 0

    def buf(self, name=None):
        self.nbuf += 1
        return Buf(name or f"b{self.nbuf}")

    def _need(self, X, tl, o, clock, waits):
        kn = self.known[X]
        if kn.get(tl, 0) >= o:
            return
        waits.append((tl, o))
        for k, v in clock.items():
            if kn.get(k, 0) < v:
                kn[k] = v

    def op(self, eng, fn, reads=(), writes=(), dma=False):
        o = Op()
        o.eng, o.fn, o.is_dma, o.idx = eng, fn, dma, len(self.ops)
        deps = set()
        for b in reads:
            if b.lw is not None:
                deps.add(b.lw)
        for b in writes:
            if b.lw is not None:
                deps.add(b.lw)
            deps.update(b.rd)
        waits = []
        if dma:
            n = self.dcnt[eng]
            self.dcnt[eng] = n + 1
            K = KSLOTS[eng]
            o.tl = ("q", eng, n % K)
            o.ord = n // K + 1
            if o.ord > 1:
                po, pc = self.last[o.tl]
                self._need(eng, o.tl, po, pc, waits)
        else:
            self.cnt[eng] += 1
            o.tl = eng
            o.ord = self.cnt[eng]
        for j in sorted(deps, reverse=True):
            d = self.ops[j]
            if d.tl == eng and not dma and eng not in self.same_sync:
                continue
            self._need(eng, d.tl, d.ord, d.clock, waits)
        o.waits = waits
        ck = dict(self.known[eng])
        ck[o.tl] = o.ord
        o.clock = ck
        self.last[o.tl] = (o.ord, ck)
        for b in reads:
            b.rd.append(o.idx)
        for b in writes:
            b.lw = o.idx
            b.rd = []
        self.ops.append(o)
        self.eng_ops[eng].append(o)
        return o

    def _sync_all(self, engines, skip_bg=False):
        lasts = dict(self.last)
        if skip_bg:
            lasts = {tl: v for tl, v in lasts.items() if not (isinstance(tl, tuple) and tl[1] == "act")}
        for e in engines:
            o = Op()
            o.eng, o.fn, o.is_dma, o.idx = e, None, False, len(self.ops)
            waits = []
            for tl, (od, ck) in lasts.items():
                self._need(e, tl, od, ck, waits)
            o.waits = waits
            o.tl, o.ord = None, 0
            o.clock = dict(self.known[e])
            self.ops.append(o)
            self.eng_ops[e].append(o)

    def barrier(self):
        self._sync_all(ENGS, skip_bg=True)

    def final_wait(self, eng="sp"):
        self._sync_all((eng,))

    def emit(self, stack):
        nc = self.nc
        waited = {e: set() for e in COMPUTE}
        for o in self.ops:
            for tl, od in o.waits:
                if isinstance(tl, str):
                    waited[tl].add(od)
        rank = {e: {od: i + 1 for i, od in enumerate(sorted(waited[e]))} for e in COMPUTE}
        sems = {}
        for e in COMPUTE:
            sems[e] = stack.enter_context(nc.semaphore(f"s_{e}"))
        for q in QUEUES:
            for k in range(KSLOTS[q]):
                sems[("q", q, k)] = stack.enter_context(nc.semaphore(f"s_{q}{k}"))
        self.stats = {e: [len(self.eng_ops[e]), 0] for e in self.eng_ops}
        block = stack.enter_context(nc.Block())
        engmap = {"pe": "tensor", "act": "scalar", "dve": "vector", "pool": "gpsimd", "sp": "sync"}

        def make(e):
            def body(engine):
                for o in self.eng_ops[e]:
                    for tl, od in o.waits:
                        if isinstance(tl, str):
                            engine.wait_ge(sems[tl], rank[tl][od])
                        else:
                            engine.wait_ge(sems[tl], 16 * od)
                        self.stats[e][1] += 1
                    if o.fn is None:
                        continue
                    ins = o.fn(engine)
                    if o.is_dma:
                        ins.then_inc(sems[o.tl], 16)
                    elif o.ord in rank[o.tl]:
                        ins.then_inc(sems[o.tl], 1)
            return body

        for e in ENGS:
            getattr(block, engmap[e])(make(e))


class Arena:
    def __init__(self, ap, words):
        self.ap = ap
        self.words = words
        self.top = 0

    def alloc(self, n_elems, dtype=F32):
        bpe = 4 if dtype in (F32, I32) else 2
        w = (n_elems * bpe + 3) // 4
        w = (w + 15) // 16 * 16
        assert self.top + w <= self.words, f"arena overflow {self.top}+{w}>{self.words}"
        v = self.ap[:, self.top:self.top + w]
        self.top += w
        if dtype != F32:
            v = v.bitcast(dtype)
        return v[:, 0:n_elems]


NCORES = 8
S = 2048
NSEQ = 2
T = NSEQ * S
NT = T // 128
D = 1024
NE = 32
CAP = 384
NSLOT = NE * CAP
TRASH = NSLOT
EPS = 1e-6
DILS = (1, 4, 16)
NEG = -30000.0


def tokslice(c, vb):
    dil = DILS[c]
    if c == 0:
        return slice(128 * vb, 128 * vb + 128, 1)
    if c == 1:
        r, n = vb // 4, vb % 4
        st = 4 * 128 * n + r
        return slice(st, st + 4 * 127 + 1, 4)
    return slice(vb, vb + 16 * 127 + 1, 16)


def prev_vb(c, vb):
    if c == 0:
        return vb - 1 if vb > 0 else None
    if c == 1:
        return vb - 1 if vb % 4 > 0 else None
    return None


def host_consts():
    bf = ml_dtypes.bfloat16
    k = np.arange(128)[:, None].astype(np.float64)
    q = np.arange(128)[None, :].astype(np.float64)
    bt = np.zeros((128, 8, 3, 2, 128), np.float32)
    for h in range(8):
        s = 2.0 ** (-(h + 1))
        for c, dil in enumerate(DILS):
            dg = np.where(q >= k, -s * dil * (q - k), NEG)
            of = np.where(k >= q, -s * dil * (q + 128 - k), NEG)
            bt[:, h, c, 0, :] = dg
            bt[:, h, c, 1, :] = of
    cons = {}
    cons["btab"] = bt.reshape(128, 48 * 128).astype(bf)
    cons["ident_bf"] = np.eye(128, dtype=np.float32).astype(bf)
    cons["ident32"] = np.eye(128, dtype=np.float32)
    cm = (k <= q).astype(np.float32)
    cons["cmask"] = np.concatenate([cm, cm], axis=1).astype(np.float32)
    sm = np.ones((128, 512), np.float32)
    sm[:, 0::128] = 0.0
    cons["scanmask"] = sm
    bd = np.zeros((128, 128), np.float32)
    bd[0:64, 0:64] = 1.0
    bd[64:128, 64:128] = 1.0
    cons["bd_bf"] = bd.astype(bf)
    cons["ones_bf"] = np.ones((128, 128), np.float32).astype(bf)
    cons["tri_bf"] = (k < q).astype(np.float32).astype(bf)
    cons["ebase"] = np.broadcast_to((np.arange(NE) * CAP).astype(np.float32)[None, :], (128, NE)).copy()
    cons["zeros_bf"] = np.zeros((NSLOT + 128, D), bf)
    cons["zeros32"] = np.zeros((128, D), np.float32)
    return cons


def build():
    nc = bass.Bass("TRN2", target_bir_lowering=False)
    P = Prog(nc)
    st = ExitStack()

    def din(name, shape, dt=F32):
        return nc.dram_tensor(name, list(shape), dt, kind="ExternalInput").ap()

    x_d = din("x", [T, D])
    win_d = din("w_in", [D, 3584])
    wout_d = din("w_out", [D, D])
    g1_d = din("g1bc", [128, D])
    g2_d = din("g2bc", [128, D])
    gf_d = din("gfbc", [128, D])
    ga_d = din("ga", [128, 4])
    gh_d = din("gh", [128, 4])
    gam_d = din("gam", [128, 8])
    wr_d = din("wr", [128, 8, 36])
    br_d = din("brbc", [128, 36])
    wg_d = din("w_gate", [NE, D, 512])
    wu_d = din("w_up", [NE, D, 512])
    wd_d = din("w_down", [NE, 512, D])
    cshape = {"btab": ([128, 48 * 128], BF16), "ident_bf": ([128, 128], BF16), "ident32": ([128, 128], F32),
              "cmask": ([128, 256], F32), "scanmask": ([128, 512], F32), "bd_bf": ([128, 128], BF16),
              "ones_bf": ([128, 128], BF16), "tri_bf": ([128, 128], BF16), "ebase": ([128, NE], F32),
              "zeros_bf": ([NSLOT + 128, D], BF16), "zeros32": ([128, D], F32)}
    cd = {k: din("c_" + k, v[0], v[1]) for k, v in cshape.items()}
    out_d = nc.dram_tensor("out", [T, D], F32, kind="ExternalOutput").ap()
    x1s = nc.dram_tensor("x1s", [T, D], F32, kind="Internal").ap()
    h2s = nc.dram_tensor("h2s", [T, D], BF16, kind="Internal").ap()
    xg = nc.dram_tensor("xg", [NSLOT + 128, D], BF16, kind="Internal").ap()
    ybuf = nc.dram_tensor("ybuf", [NSLOT + 128, D], BF16, kind="Internal").ap()
    b_x1s = [P.buf() for _ in range(NT)]
    b_h2s = [P.buf() for _ in range(NT)]
    vsd = [nc.dram_tensor(f"vsd{i}", [S, 128], BF16, kind="Internal").ap() for i in range(2)]
    b_vs = [P.buf() for _ in range(2)]
    b_xg = P.buf("xg")
    b_xgz = [P.buf() for _ in range(4)]
    b_xge = [P.buf() for _ in range(NE)]
    b_ybuf = P.buf("ybuf")
    b_out = P.buf("out")

    AW = 53000
    arena_t = st.enter_context(nc.sbuf_tensor("arena", [128, AW], F32))
    A = Arena(arena_t[:, :], AW)
    pbank = [st.enter_context(nc.psum_tensor(f"pb{i}", [128, 512], F32)) for i in range(8)]
    pbuf = [P.buf(f"pb{i}") for i in range(8)]

    def PS(i, dt=F32):
        v = pbank[i][:, :]
        return v.bitcast(BF16) if dt == BF16 else v

    rot_state = {}

    def rot(name, banks):
        i = rot_state.get(name, 0)
        rot_state[name] = i + 1
        return banks[i % len(banks)]

    def dma(q, out, in_, reads=(), writes=()):
        P.op(q, lambda e: e.dma_start(out=out, in_=in_), reads, writes, dma=True)

    def mm(out, lhsT, rhs, start, stop, reads, writes):
        P.op("pe", lambda e: e.matmul(out, lhsT=lhsT, rhs=rhs, start=start, stop=stop), reads, writes)

    def tr(out, in_, ident, reads, writes):
        P.op("pe", lambda e: e.transpose(out=out, in_=in_, identity=ident), reads, writes)

    def act(out, in_, func, reads, writes, bias=None, scale=None, accum=None):
        kw = {}
        if bias is not None:
            kw["bias"] = bias
        if scale is not None:
            kw["scale"] = scale
        if accum is not None:
            kw["accum_out"] = accum
        P.op("act", lambda e: e.activation(out=out, in_=in_, func=func, **kw), reads, writes)

    def tt(eng, out, in0, in1, op, reads, writes):
        P.op(eng, lambda e: e.tensor_tensor(out=out, in0=in0, in1=in1, op=op), reads, writes)

    def ts(eng, out, in0, s1, s2, op0, op1, reads, writes):
        if s2 is None:
            P.op(eng, lambda e: e.tensor_scalar(out=out, in0=in0, scalar1=s1, scalar2=None, op0=op0), reads, writes)
        else:
            P.op(eng, lambda e: e.tensor_scalar(out=out, in0=in0, scalar1=s1, scalar2=s2, op0=op0, op1=op1), reads, writes)

    def stt(eng, out, in0, scalar, in1, op0, op1, reads, writes):
        P.op(eng, lambda e: e.scalar_tensor_tensor(out=out, in0=in0, scalar=scalar, in1=in1, op0=op0, op1=op1),
             reads, writes)

    def cp(eng, out, in_, reads, writes):
        if eng == "act":
            act(out, in_, AF.Copy, reads, writes)
        else:
            P.op(eng, lambda e: e.tensor_copy(out=out, in_=in_), reads, writes)

    def rsqrt_mean(out, in_, n, reads, writes):
        act(out, in_, AF.Ln, reads, writes, bias=epsc[:, 0:1], scale=1.0 / n)
        act(out, out, AF.Exp, writes, writes, scale=-0.5)

    C = {}
    bC = P.buf("consts")
    for k, (shp, dt) in cshape.items():
        if k in ("zeros_bf", "btab", "zeros32"):
            continue
        C[k] = A.alloc(shp[1], dt)
        dma("sp", C[k], cd[k], writes=[bC])
    epsc = A.alloc(1)
    P.op("pool", lambda e: e.memset(epsc, EPS), writes=[bC])
    g1bc = A.alloc(D); dma("sp", g1bc, g1_d, writes=[bC])
    ga = A.alloc(4); dma("sp", ga, ga_d, writes=[bC])
    gh = A.alloc(4); dma("sp", gh, gh_d, writes=[bC])
    gam = A.alloc(8); dma("sp", gam, gam_d, writes=[bC])
    lb = A.alloc(4); oml = A.alloc(4); noml = A.alloc(4)
    tt("dve", lb, gam[:, 0:4], gam[:, 4:8], ALU.subtract, [bC], [bC])
    act(lb, lb, AF.Sigmoid, [bC], [bC])
    ts("dve", oml, lb, -1.0, 1.0, ALU.mult, ALU.add, [bC], [bC])
    ts("dve", noml, oml, -1.0, None, ALU.mult, None, [bC], [bC])
    Lall = A.alloc(NT * 36)
    bL = P.buf("Lall")
    top_persist = A.top

    win = A.alloc(8 * 3584, BF16); bwin = P.buf("win")
    win3 = win.rearrange("p (c n) -> p c n", c=8)
    g2bc = A.alloc(D); dma("sp", g2bc, g2_d, writes=[bC])
    wr = A.alloc(8 * 36); dma("sp", wr, wr_d.rearrange("p c n -> p (c n)"), writes=[bC])
    wr3 = wr.rearrange("p (c n) -> p c n", c=8)
    brbc = A.alloc(36); dma("sp", brbc, br_d, writes=[bC])
    bwin_h = P.buf("win_h")
    for c in range(8):
        dma("pool", win3[:, c, 0:1536], win_d[c * 128:(c + 1) * 128, 0:1536], writes=[bwin])
    for c in range(8):
        dma("pool", win3[:, c, 1536:3584], win_d[c * 128:(c + 1) * 128, 1536:3584], writes=[bwin_h])
    hT = A.alloc(8 * S, BF16); bhT = P.buf("hT")
    hT3 = hT.rearrange("p (c t) -> p c t", c=8)
    mixT = A.alloc(8 * S, BF16)
    mixT3 = mixT.rearrange("p (c t) -> p c t", c=8)
    bmix = [P.buf(f"mix{i}") for i in range(8)]
    scratch = A.top

    ident_bf, ident32 = C["ident_bf"], C["ident32"]
    ones_bf = C["ones_bf"]

    for sq in range(NSEQ):
        if sq > 0:
            P.barrier()
        A.top = scratch
        NX = 4
        xt = [A.alloc(D) for _ in range(NX)]; bxt = [P.buf() for _ in range(NX)]
        hb = [A.alloc(D, BF16) for _ in range(NX)]; bhb = [P.buf() for _ in range(NX)]
        sm1 = [A.alloc(2) for _ in range(NX)]; bsm1 = [P.buf() for _ in range(NX)]
        junk = A.alloc(D, BF16); bjunk = P.buf("junk")
        for t in range(NX - 1):
            dma("sp", xt[t], x_d[(sq * 16 + t) * 128:(sq * 16 + t + 1) * 128, :], writes=[bxt[t]])
        for t in range(16):
            gt = sq * 16 + t
            i = t % NX
            if t + NX - 1 < 16:
                t2 = t + NX - 1
                dma("sp", xt[t2 % NX], x_d[(sq * 16 + t2) * 128:(sq * 16 + t2 + 1) * 128, :], writes=[bxt[t2 % NX]])
            act(junk, xt[i], AF.Square, [bxt[i]], [bjunk, bsm1[i]], accum=sm1[i][:, 0:1])
            rsqrt_mean(sm1[i][:, 0:1], sm1[i][:, 0:1], D, [bsm1[i], bC], [bsm1[i]])
            stt("dve", hb[i], xt[i], sm1[i][:, 0:1], g1bc, ALU.mult, ALU.mult, [bxt[i], bsm1[i], bC], [bhb[i]])
            pb = rot("tr", [0, 1])
            for c in range(8):
                tr(PS(pb, BF16)[:, c * 128:(c + 1) * 128], hb[i][:, c * 128:(c + 1) * 128], ident_bf,
                   [bhb[i], bC], [pbuf[pb]])
            cp("act" if t % 2 else "dve", hT3[:, :, t * 128:(t + 1) * 128],
               PS(pb, BF16).rearrange("p (c t) -> p c t", c=8), [pbuf[pb]], [bhT])

        def proj_fm(col0, cb):
            pb = rot("proj", [0, 1])
            for c in range(8):
                mm(PS(pb), win3[:, c, col0:col0 + 128], hT3[:, c, cb * 512:(cb + 1) * 512], c == 0, c == 7,
                   [bwin if col0 < 1536 else bwin_h, bhT], [pbuf[pb]])
            return pb

        P.barrier()
        A.top = scratch
        QT = A.alloc(S, BF16); KT = A.alloc(S, BF16); bQT = P.buf(); bKT = P.buf()
        VcS = [[A.alloc(16 * 128, BF16) for _ in range(3)] for _ in range(2)]
        bVcS = [[P.buf() for _ in range(3)] for _ in range(2)]
        Uacc = A.alloc(S); Zacc = A.alloc(S); bUacc = P.buf(); bZacc = P.buf()
        ssacc = A.alloc(S); bss = P.buf()
        PT = [A.alloc(512, BF16) for _ in range(3)]; bPT = [P.buf() for _ in range(3)]
        osq = [A.alloc(512, BF16) for _ in range(2)]; bosq = [P.buf() for _ in range(2)]
        btab = A.alloc(12 * 128, BF16); bbt = P.buf()
        btab3 = btab.rearrange("p (j q) -> p j q", q=128)
        def vproj_hp(hp_):
            k_ = hp_ % 2
            Vn = VcS[k_][0]
            for g in range(4):
                pb = rot("proj", [0, 1])
                for j in range(4):
                    tl = g * 4 + j
                    for ch in range(8):
                        mm(PS(pb)[:, j * 128:(j + 1) * 128], hT3[:, ch, tl * 128:(tl + 1) * 128],
                           win3[:, ch, 1024 + hp_ * 128:1024 + (hp_ + 1) * 128], ch == 0, ch == 7,
                           [bwin, bhT], [pbuf[pb]])
                cp("act" if g % 2 else "dve", Vn[:, g * 512:(g + 1) * 512], PS(pb), [pbuf[pb]], [bVcS[k_][0]])
            dma("sp", vsd[k_].rearrange("(v p) f -> p v f", p=128), Vn.rearrange("p (v f) -> p v f", f=128),
                reads=[bVcS[k_][0]], writes=[b_vs[k_]])
            for r in range(4):
                dma("sp", VcS[k_][1].rearrange("p (r n f) -> p r n f", r=4, n=4)[:, r, :, :],
                    vsd[k_].rearrange("(n p r) f -> p r n f", n=4, p=128, r=4)[:, r, :, :],
                    reads=[b_vs[k_]], writes=[bVcS[k_][1]])
            dma("sp", VcS[k_][2].rearrange("p (r f) -> p r f", f=128), vsd[k_].rearrange("(p r) f -> p r f", r=16),
                reads=[b_vs[k_]], writes=[bVcS[k_][2]])

        vproj_hp(0)
        for hp in range(4):
            Vc, bVc = VcS[hp % 2], bVcS[hp % 2]
            dma("sp", btab, cd["btab"][:, hp * 1536:(hp + 1) * 1536], writes=[bbt])
            for cb in range(4):
                pb = proj_fm(hp * 128, cb)
                ts("dve", QT[:, cb * 512:(cb + 1) * 512], PS(pb), 0.125, None, ALU.mult, None, [pbuf[pb]], [bQT])
                pb = proj_fm(512 + hp * 128, cb)
                cp("act", KT[:, cb * 512:(cb + 1) * 512], PS(pb), [pbuf[pb]], [bKT])
            if hp + 1 < 4:
                vproj_hp(hp + 1)
            def att_scores(c, vb):
                pv = prev_vb(c, vb)
                kbs = [vb] + ([pv] if pv is not None else [])
                qs = tokslice(c, vb)
                pS = rot("S", [2, 3])
                ip = rot("PT", [0, 1, 2])
                for ty, kb in enumerate(kbs):
                    ks = tokslice(c, kb)
                    for e in range(2):
                        reg = PS(pS)[:, (ty * 2 + e) * 128:(ty * 2 + e + 1) * 128]
                        mm(reg, KT[64 * e:64 * e + 64, ks], QT[64 * e:64 * e + 64, qs], True, False,
                           [bKT, bQT], [pbuf[pS]])
                        mm(reg, ident_bf, btab3[:, (e * 3 + c) * 2 + ty, :], False, True, [bC, bbt], [pbuf[pS]])
                n = 256 * len(kbs)
                act(PT[ip][:, 0:n], PS(pS)[:, 0:n], AF.Exp, [pbuf[pS]], [bPT[ip]])
                return ip, kbs

            def att_pv(c, g, j, pU, pZ, ip, kbs):
                Vc3 = Vc[c].rearrange("p (v f) -> p v f", f=128)
                for e in range(2):
                    for ty, kb in enumerate(kbs):
                        mm(PS(pU)[64 * e:64 * e + 64, j * 128:(j + 1) * 128], Vc3[:, kb, 64 * e:64 * e + 64],
                           PT[ip][:, (ty * 2 + e) * 128:(ty * 2 + e + 1) * 128], ty == 0, ty == len(kbs) - 1,
                           [bVc[c], bPT[ip]], [pbuf[pU]])
                    for ty, kb in enumerate(kbs):
                        mm(PS(pZ)[64 * e:64 * e + 64, j * 128:(j + 1) * 128], ones_bf[:, 0:64],
                           PT[ip][:, (ty * 2 + e) * 128:(ty * 2 + e + 1) * 128], ty == 0, ty == len(kbs) - 1,
                           [bC, bPT[ip]], [pbuf[pZ]])
                if j == 3:
                    for (acc, bacc, pb_) in ((Uacc, bUacc, pU), (Zacc, bZacc, pZ)):
                        if c == 0:
                            dst = acc[:, g * 512:(g + 1) * 512]
                            src = PS(pb_)
                        elif c == 1:
                            dst = acc[:, g:g + 4 * 511 + 1:4]
                            src = PS(pb_)
                        else:
                            dst = acc.rearrange("a (p r) -> a r p", r=16)[:, 4 * g:4 * g + 4, :]
                            src = PS(pb_).rearrange("a (j p) -> a j p", j=4)
                        if c == 0:
                            cp("dve", dst, src, [pbuf[pb_]], [bacc])
                        else:
                            tt("dve", dst, dst, src, ALU.add, [pbuf[pb_], bacc], [bacc])

            pend = None
            for c in range(3):
                for g in range(4):
                    pU = rot("U", [4, 5])
                    pZ = rot("Z", [6, 7])
                    for j in range(4):
                        ip, kbs = att_scores(c, g * 4 + j)
                        if pend is not None:
                            att_pv(*pend)
                        pend = (c, g, j, pU, pZ, ip, kbs)
            att_pv(*pend)
            act(Zacc, Zacc, AF.Ln, [bZacc], [bZacc])
            act(Zacc, Zacc, AF.Exp, [bZacc], [bZacc], scale=-1.0)
            tt("dve", Uacc, Uacc, Zacc, ALU.mult, [bUacc, bZacc], [bUacc])
            cp("pool", mixT3[:, hp, :], Uacc, [bUacc], [bmix[hp]])
            for cb in range(4):
                io = rot("osq", [0, 1])
                act(osq[io], Uacc[:, cb * 512:(cb + 1) * 512], AF.Square, [bUacc], [bosq[io]])
                pb = rot("proj", [0, 1])
                mm(PS(pb), ones_bf, osq[io], True, True, [bC, bosq[io]], [pbuf[pb]])
                if hp == 0:
                    cp("dve", ssacc[:, cb * 512:(cb + 1) * 512], PS(pb), [pbuf[pb]], [bss])
                else:
                    tt("dve", ssacc[:, cb * 512:(cb + 1) * 512], ssacc[:, cb * 512:(cb + 1) * 512], PS(pb), ALU.add,
                       [pbuf[pb], bss], [bss])
        rsqrt_mean(ssacc, ssacc, 512, [bss, bC], [bss])
        for hp in range(4):
            stt("dve", mixT3[:, hp, :], mixT3[:, hp, :], ga[:, hp:hp + 1], ssacc, ALU.mult, ALU.mult,
                [bmix[hp], bss, bC], [bmix[hp]])

        P.barrier()
        A.top = scratch
        qinT = A.alloc(S, BF16); kinT = A.alloc(S, BF16); kendT = A.alloc(S, BF16); gsT = A.alloc(S, BF16)
        bqin = P.buf(); bkin = P.buf(); bkend = P.buf(); bgs = P.buf()
        iV = A.alloc(16 * 128, BF16); biV = P.buf()
        dec = A.alloc(16); bdec = P.buf()
        sigf = A.alloc(S); bsigf = [P.buf() for _ in range(2)]
        tBf = A.alloc(S); btB = [P.buf() for _ in range(2)]
        tEf = A.alloc(S); btE = [P.buf() for _ in range(2)]
        tCf = A.alloc(S); btC = [P.buf() for _ in range(2)]
        qsT = A.alloc(S, BF16); bqs = P.buf()
        Am = [A.alloc(256, BF16) for _ in range(2)]; bAmh = [[P.buf(), P.buf()] for _ in range(2)]
        ket = [A.alloc(512, BF16) for _ in range(2)]; bket = [P.buf() for _ in range(2)]
        osq = [A.alloc(512, BF16) for _ in range(2)]; bosq = [P.buf() for _ in range(2)]
        decm = A.alloc(1024); bdecm = P.buf()
        Sall = A.alloc(1024, BF16); bSall = P.buf()
        Sall3 = Sall.rearrange("p (v t) -> p v t", t=16)
        if sq == 0:
            nr = (NSLOT + 128) // 4
            for i in range(4):
                dma("act", xg[i * nr:(i + 1) * nr, :], cd["zeros_bf"][i * nr:(i + 1) * nr, :], writes=[b_xgz[i]])
            dma("act", ybuf[NSLOT:NSLOT + 128, :], cd["zeros_bf"][0:128, :], writes=[b_ybuf])
        for hp in range(4):
            cq, cf, ci, cg = 1536 + hp * 128, 2048 + hp * 128, 2560 + hp * 128, 3072 + hp * 128
            for cb in range(4):
                cs = slice(cb * 512, (cb + 1) * 512)
                pf = proj_fm(cf, cb)
                act(sigf[:, cs], PS(pf), AF.Sigmoid, [pbuf[pf]], [bsigf[cb // 2]])
            for cb in range(4):
                cs = slice(cb * 512, (cb + 1) * 512)
                pq = proj_fm(cq, cb)
                act(qsT[:, cs], PS(pq), AF.Silu, [pbuf[pq]], [bqs])
                pg = proj_fm(cg, cb)
                act(gsT[:, cs], PS(pg), AF.Silu, [pbuf[pg]], [bgs])
            HS = [slice(0, 1024), slice(1024, 2048)]
            for h in range(2):
                ts("dve", tBf[:, HS[h]], sigf[:, HS[h]], oml[:, hp:hp + 1], lb[:, hp:hp + 1], ALU.mult, ALU.add,
                   [bsigf[h], bC], [btB[h]])
            for h in range(2):
                act(tBf[:, HS[h]], tBf[:, HS[h]], AF.Ln, [btB[h]], [btB[h]])
            for h in range(2):
                ts("dve", sigf[:, HS[h]], sigf[:, HS[h]], noml[:, hp:hp + 1], oml[:, hp:hp + 1], ALU.mult, ALU.add,
                   [bsigf[h], bC], [bsigf[h]])
            for h in range(2):
                for j in range(2):
                    c5 = slice(h * 1024 + j * 512, h * 1024 + (j + 1) * 512)
                    P.op("dve", lambda e, o_=tEf[:, c5], d_=tBf[:, c5], m_=C["scanmask"]: e.tensor_tensor_scan(
                        out=o_, data0=m_, data1=d_, initial=0.0, op0=ALU.mult, op1=ALU.add), [btB[h], bC], [btE[h]])
            for h in range(2):
                act(tCf[:, HS[h]], tEf[:, HS[h]], AF.Exp, [btE[h]], [btC[h]])
            for h in range(2):
                tt("dve", qinT[:, HS[h]], qsT[:, HS[h]], tCf[:, HS[h]], ALU.mult, [bqs, btC[h]], [bqin])
            for h in range(2):
                act(tCf[:, HS[h]], tEf[:, HS[h]], AF.Exp, [btE[h], btC[h]], [btC[h]], scale=-1.0)
            act(dec[:, 0:16], tEf[:, 127:2048:128], AF.Exp, [btE[0], btE[1]], [bdec])
            for h in range(2):
                tt("dve", kinT[:, HS[h]], sigf[:, HS[h]], tCf[:, HS[h]], ALU.mult, [bsigf[h], btC[h]], [bkin])
            for h in range(2):
                tt("dve", kendT[:, HS[h]].rearrange("p (a b) -> p a b", b=128), kinT[:, HS[h]].rearrange("p (a b) -> p a b", b=128),
                   dec[:, h * 8:(h + 1) * 8].unsqueeze(2).to_broadcast([128, 8, 128]), ALU.mult, [bkin, bdec], [bkend])
            for g in range(4):
                pb = g
                for j in range(4):
                    tl = g * 4 + j
                    for ch in range(8):
                        mm(PS(pb)[:, j * 128:(j + 1) * 128], hT3[:, ch, tl * 128:(tl + 1) * 128], win3[:, ch, ci:ci + 128],
                           ch == 0, ch == 7, [bwin_h, bhT], [pbuf[pb]])
                cp("act" if g % 2 else "dve", iV[:, g * 512:(g + 1) * 512], PS(pb), [pbuf[pb]], [biV])
            iV3 = iV.rearrange("p (v f) -> p v f", f=128)
            cp("pool", decm.rearrange("p (v t) -> p v t", t=16), dec[:, 0:16].unsqueeze(1).to_broadcast([128, 64, 16]),
               [bdec], [bdecm])
            P.op("pool", lambda e: e.memset(decm[:, 0:1024:16], 0.0), [bdecm], [bdecm])
            for g4 in range(4):
                i = g4 % 2
                pt_ = 4 + i
                for j in range(4):
                    tk = g4 * 4 + j
                    tr(PS(pt_, BF16)[:, j * 128:(j + 1) * 128], kendT[:, tk * 128:(tk + 1) * 128], ident_bf, [bkend, bC], [pbuf[pt_]])
                cp("act" if i else "dve", ket[i], PS(pt_, BF16)[:, 0:512], [pbuf[pt_]], [bket[i]])
                for j in range(4):
                    tk = g4 * 4 + j
                    for e in range(2):
                        for vh in range(2):
                            mm(PS(vh)[64 * e:64 * e + 64, tk:512:16], ket[i][:, j * 128 + 64 * e:j * 128 + 64 * e + 64],
                               iV3[:, tk, 64 * e + 32 * vh:64 * e + 32 * vh + 32], True, True, [bket[i], biV], [pbuf[vh]])
            for vh in range(2):
                P.op("dve", lambda e, o_=Sall[:, vh * 512:(vh + 1) * 512], d0=decm[:, vh * 512:(vh + 1) * 512], d1=PS(vh):
                     e.tensor_tensor_scan(out=o_, data0=d0, data1=d1, initial=0.0, op0=ALU.mult, op1=ALU.add),
                     [bdecm, pbuf[vh]], [bSall])

            def stage1(tk):
                i = tk % 2
                cs = slice(tk * 128, (tk + 1) * 128)
                ab_ = 2 if tk % 2 == 0 else 0
                for e in range(2):
                    mm(PS(ab_ + e)[:, 0:128], kinT[64 * e:64 * e + 64, cs], qinT[64 * e:64 * e + 64, cs], True, True,
                       [bkin, bqin], [pbuf[ab_ + e]])
                for e in range(2):
                    tt("dve", Am[i][:, e * 128:(e + 1) * 128], PS(ab_ + e)[:, 0:128], C["cmask"][:, 0:128], ALU.mult,
                       [pbuf[ab_ + e], bC], [bAmh[i][e]])

            def stage2(tk, po):
                i = tk % 2
                cs = slice(tk * 128, (tk + 1) * 128)
                j = tk % 4
                for e in range(2):
                    mm(PS(po)[64 * e:64 * e + 64, j * 128:(j + 1) * 128], iV3[:, tk, 64 * e:64 * e + 64],
                       Am[i][:, e * 128:(e + 1) * 128], True, tk == 0, [biV, bAmh[i][e]], [pbuf[po]])
                    if tk > 0:
                        mm(PS(po)[64 * e:64 * e + 64, j * 128:(j + 1) * 128], Sall3[64 * e:64 * e + 64, :, tk - 1],
                           qinT[64 * e:64 * e + 64, cs], False, True, [bSall, bqin], [pbuf[po]])

            def post_a(cb, po):
                io = cb % 2
                act(osq[io], PS(po), AF.Square, [pbuf[po]], [bosq[io]])

            def post_b(cb, po):
                io = cb % 2
                tmp = tBf[:, cb * 512:(cb + 1) * 512]
                mm(PS(5), C["bd_bf"], osq[io], True, True, [bC, bosq[io]], [pbuf[5]])
                rsqrt_mean(tmp, PS(5), 64, [pbuf[5], bC], [btB[cb // 2]])

            def post_c(cb, po):
                cs = slice(cb * 512, (cb + 1) * 512)
                tmp = tBf[:, cs]
                stt("dve", tmp, PS(po), gh[:, hp:hp + 1], tmp, ALU.mult, ALU.mult, [pbuf[po], bC, btB[cb // 2]], [btB[cb // 2]])
                tt("pool", mixT3[:, 4 + hp, cs], tmp, gsT[:, cs], ALU.mult, [btB[cb // 2], bgs], [bmix[4 + hp]])

            po = None
            pos_ = {}
            stage1(0)
            for tk in range(16):
                if tk + 1 < 16:
                    stage1(tk + 1)
                if tk % 4 == 0:
                    po = rot("O", [6, 7])
                    pos_[tk // 4] = po
                stage2(tk, po)
                g_ = tk // 4
                if tk % 4 == 3:
                    post_a(g_, po)
                if tk % 4 == 0 and g_ >= 1:
                    post_b(g_ - 1, pos_[g_ - 1])
                if tk % 4 == 2 and g_ >= 1:
                    post_c(g_ - 1, pos_[g_ - 1])
            post_b(3, pos_[3])
            post_c(3, pos_[3])

        P.barrier()
        A.top = scratch
        wout = A.alloc(8 * D, BF16); bwout = P.buf("wout")
        wout3 = wout.rearrange("p (c n) -> p c n", c=8)
        dma("pool", wout3, wout_d.rearrange("(c p) n -> p c n", p=128), writes=[bwout])
        xt = [A.alloc(D) for _ in range(2)]; bxt = [P.buf() for _ in range(2)]
        sm1 = [A.alloc(2) for _ in range(2)]; bsm1 = [P.buf() for _ in range(2)]
        junk = A.alloc(D, BF16); bjunk = P.buf("junk")
        x1t = [A.alloc(D) for _ in range(2)]; bx1 = [P.buf() for _ in range(2)]
        h2f = [A.alloc(D) for _ in range(2)]; bh2f = [P.buf() for _ in range(2)]
        h2b = [A.alloc(D, BF16) for _ in range(2)]; bh2b = [P.buf() for _ in range(2)]
        h2T = [A.alloc(8 * 128) for _ in range(2)]; bh2T = [P.buf() for _ in range(2)]
        def op_a(t):
            gt = sq * 16 + t
            i = gt % 2
            pa, pb2 = (0, 1) if i == 0 else (2, 3)
            dma("sp", xt[i], x_d[gt * 128:(gt + 1) * 128, :], writes=[bxt[i]])
            for half, pb in ((0, pa), (1, pb2)):
                for c in range(8):
                    mm(PS(pb), mixT3[:, c, t * 128:(t + 1) * 128], wout3[:, c, half * 512:(half + 1) * 512], c == 0, c == 7,
                       [bmix[c], bwout], [pbuf[pb]])
                tt("dve", x1t[i][:, half * 512:(half + 1) * 512], xt[i][:, half * 512:(half + 1) * 512], PS(pb), ALU.add,
                   [bxt[i], pbuf[pb]], [bx1[i]])
            dma("sp", x1s[gt * 128:(gt + 1) * 128, :], x1t[i], reads=[bx1[i]], writes=[b_x1s[gt]])
            act(junk, x1t[i], AF.Square, [bx1[i]], [bjunk, bsm1[i]], accum=sm1[i][:, 0:1])
            rsqrt_mean(sm1[i][:, 0:1], sm1[i][:, 0:1], D, [bsm1[i], bC], [bsm1[i]])
            stt("dve", h2f[i], x1t[i], sm1[i][:, 0:1], g2bc, ALU.mult, ALU.mult, [bx1[i], bsm1[i], bC], [bh2f[i]])
            cp("pool", h2b[i], h2f[i], [bh2f[i]], [bh2b[i]])
            dma("sp", h2s[gt * 128:(gt + 1) * 128, :], h2b[i], reads=[bh2b[i]], writes=[b_h2s[gt]])

        def op_b(t):
            gt = sq * 16 + t
            i = gt % 2
            for c in range(8):
                pbt = 4 + c // 4
                tr(PS(pbt)[:, (c % 4) * 128:(c % 4 + 1) * 128], h2f[i][:, c * 128:(c + 1) * 128], ident32, [bh2f[i], bC],
                   [pbuf[pbt]])
            cp("act", h2T[i][:, 0:512], PS(4), [pbuf[4]], [bh2T[i]])
            cp("act", h2T[i][:, 512:1024], PS(5), [pbuf[5]], [bh2T[i]])
            pl = rot("L", [6, 7])
            for c in range(8):
                mm(PS(pl)[:, 0:36], h2T[i][:, c * 128:(c + 1) * 128], wr3[:, c, :], c == 0, c == 7, [bh2T[i], bC], [pbuf[pl]])
            tt("dve", Lall[:, gt * 36:(gt + 1) * 36], PS(pl)[:, 0:36], brbc, ALU.add, [pbuf[pl], bC], [bL])

        op_a(0)
        for t in range(16):
            if t + 1 < 16:
                op_a(t + 1)
            op_b(t)

    P.barrier()
    A.top = top_persist
    L3 = Lall.rearrange("p (t n) -> p t n", n=36)

    def al(n, dt=F32):
        return A.alloc(n, dt)
    bR = P.buf("route")
    gate1 = al(NT); gate2 = al(NT); d1i = al(NT, I32); d2i = al(NT, I32)
    top_route = A.top
    gmax = al(NT); goh = al(NT * 4); gexp = al(NT * 4); gsum = al(NT); wgt = al(NT)
    sel = al(NT * 8); tmp8 = al(NT * 8); m1 = al(NT); m2 = al(NT); oh1 = al(NT * 8); oh2 = al(NT * 8)
    dd = al(NT)
    A1 = al(NT * 32); A2 = al(NT * 32); Aall = al(NT * 32, BF16); pos = al(NT * 32); tot = al(NT * 32)
    cum = al(NT * 32); tmp32 = al(NT * 32); valid = al(NT * 32)
    d1f = al(NT); d2f = al(NT)
    RW = [bL, bR, bC]

    def v3(ap, n):
        return ap.rearrange("p (t n) -> p t n", n=n)

    def bc(ap, n):
        return ap.unsqueeze(2).to_broadcast([128, NT, n])

    P.op("dve", lambda e: e.tensor_reduce(out=gmax, in_=L3[:, :, 0:4], axis=AX.X, op=ALU.max), RW, RW)
    tt("dve", v3(goh, 4), L3[:, :, 0:4], bc(gmax, 4), ALU.is_equal, RW, RW)
    tt("dve", v3(gexp, 4), L3[:, :, 0:4], bc(gmax, 4), ALU.subtract, RW, RW)
    act(gexp, gexp, AF.Exp, RW, RW)
    P.op("dve", lambda e: e.tensor_reduce(out=gsum, in_=v3(gexp, 4), axis=AX.X, op=ALU.add), RW, RW)
    P.op("dve", lambda e: e.reciprocal(out=wgt, in_=gsum), RW, RW)
    for g in range(4):
        src = L3[:, :, 4 + 8 * g:12 + 8 * g]
        gsel = v3(goh, 4)[:, :, g:g + 1].to_broadcast([128, NT, 8])
        if g == 0:
            tt("dve", v3(sel, 8), src, gsel, ALU.mult, RW, RW)
        else:
            tt("dve", v3(tmp8, 8), src, gsel, ALU.mult, RW, RW)
            tt("dve", sel, sel, tmp8, ALU.add, RW, RW)
    P.op("dve", lambda e: e.tensor_reduce(out=m1, in_=v3(sel, 8), axis=AX.X, op=ALU.max), RW, RW)
    tt("dve", v3(oh1, 8), v3(sel, 8), bc(m1, 8), ALU.is_equal, RW, RW)
    stt("dve", tmp8, oh1, -1e30, sel, ALU.mult, ALU.add, RW, RW)
    P.op("dve", lambda e: e.tensor_reduce(out=m2, in_=v3(tmp8, 8), axis=AX.X, op=ALU.max), RW, RW)
    tt("dve", v3(oh2, 8), v3(tmp8, 8), bc(m2, 8), ALU.is_equal, RW, RW)
    tt("dve", dd, m2, m1, ALU.subtract, RW, RW)
    act(dd, dd, AF.Exp, RW, RW)
    ts("dve", dd, dd, 1.0, None, ALU.add, None, RW, RW)
    P.op("dve", lambda e: e.reciprocal(out=dd, in_=dd), RW, RW)
    tt("dve", gate1, wgt, dd, ALU.mult, RW, RW)
    tt("dve", gate2, wgt, gate1, ALU.subtract, RW, RW)
    for (Ak, ohk) in ((A1, oh1), (A2, oh2)):
        tt("dve", Ak.rearrange("p (t g j) -> p t g j", g=4, j=8),
           v3(goh, 4).unsqueeze(3).to_broadcast([128, NT, 4, 8]),
           v3(ohk, 8).unsqueeze(2).to_broadcast([128, NT, 4, 8]), ALU.mult, RW, RW)
    tt("dve", Aall, A1, A2, ALU.add, RW, RW)
    for hf in range(2):
        sl = slice(hf * 512, (hf + 1) * 512)
        mm(PS(hf), C["tri_bf"], Aall[:, sl], True, True, RW, [pbuf[hf]])
        mm(PS(2 + hf), ones_bf, Aall[:, sl], True, True, RW, [pbuf[2 + hf]])
        cp("dve", pos[:, sl], PS(hf), [pbuf[hf]], RW)
        cp("dve", tot[:, sl], PS(2 + hf), [pbuf[2 + hf]], RW)
    P.op("dve", lambda e: e.memset(cum[:, 0:32], 0.0), RW, RW)
    for t in range(1, NT):
        tt("dve", cum[:, t * 32:(t + 1) * 32], cum[:, (t - 1) * 32:t * 32], tot[:, (t - 1) * 32:t * 32], ALU.add, RW, RW)
    tt("dve", pos, pos, cum, ALU.add, RW, RW)
    ts("dve", valid, pos, float(CAP), None, ALU.is_lt, None, RW, RW)
    tt("dve", v3(pos, 32), v3(pos, 32), C["ebase"].unsqueeze(1).to_broadcast([128, NT, 32]), ALU.add, RW, RW)
    ts("dve", pos, pos, float(-TRASH), None, ALU.add, None, RW, RW)
    tt("dve", pos, pos, valid, ALU.mult, RW, RW)
    for (Ak, dkf, dki) in ((A1, d1f, d1i), (A2, d2f, d2i)):
        tt("dve", tmp32, pos, Ak, ALU.mult, RW, RW)
        P.op("dve", lambda e, o_=dkf: e.tensor_reduce(out=o_, in_=v3(tmp32, 32), axis=AX.X, op=ALU.add), RW, RW)
        ts("dve", dkf, dkf, float(TRASH), None, ALU.add, None, RW, RW)
        cp("dve", dki, dkf, RW, RW)

    P.barrier()
    A.top = top_route
    NH = 6
    b_sc = [P.buf() for _ in range(2 * NT)]
    b_sc_used = []
    _hs_top = A.top
    hsb = [A.alloc(D, BF16) for _ in range(NH)]; bhsb = [P.buf() for _ in range(NH)]
    for gt in range(NT):
        i = gt % NH
        dma("sp", hsb[i], h2s[gt * 128:(gt + 1) * 128, :], reads=[b_h2s[gt]], writes=[bhsb[i]])
        for dki in (d1i, d2i):
            P.op("pool", lambda e, s_=hsb[i], o_=dki[:, gt:gt + 1]: e.indirect_dma_start(
                out=xg, out_offset=bass.IndirectOffsetOnAxis(ap=o_, axis=0), in_=s_, in_offset=None),
                [bhsb[i], bR] + b_xgz, [b_sc[len(b_sc_used)]], dma=True)
            b_sc_used.append(1)

    A.top = _hs_top
    XT = [A.alloc(8 * CAP, BF16) for _ in range(2)]; bXT = [P.buf() for _ in range(2)]
    assert A.top >= _hs_top + NH * (D // 2)
    wgb = [A.alloc(8 * 512, BF16) for _ in range(2)]; bwg = [P.buf() for _ in range(2)]
    wub = [A.alloc(8 * 512, BF16) for _ in range(2)]; bwu = [P.buf() for _ in range(2)]
    wdb = [A.alloc(4 * D, BF16) for _ in range(2)]; bwd = [P.buf() for _ in range(2)]
    stage = [[A.alloc(4096) for _ in range(3)] for _ in range(2)]; bstg = [[P.buf() for _ in range(3)] for _ in range(2)]
    NST = CAP // 128
    xgt = [A.alloc(NST * D, BF16) for _ in range(2)]; bxgt = [P.buf() for _ in range(2)]
    sg = [A.alloc(CAP) for _ in range(2)]; bsg = [P.buf() for _ in range(2)]
    hidT = [A.alloc(4 * CAP, BF16) for _ in range(2)]; bhid = [P.buf() for _ in range(2)]
    yt = [A.alloc(D, BF16) for _ in range(3)]; byt = [P.buf() for _ in range(3)]

    b_ys = []

    def loads(ex):
        k = ex % 2
        dma("sp", stage[k][0].rearrange("p (c n) -> p c n", c=8), wg_d[ex].rearrange("(c p) n -> p c n", p=128), writes=[bstg[k][0]])
        dma("sp", stage[k][1].rearrange("p (c n) -> p c n", c=8), wu_d[ex].rearrange("(c p) n -> p c n", p=128), writes=[bstg[k][1]])
        dma("sp", stage[k][2].rearrange("p (c n) -> p c n", c=4), wd_d[ex].rearrange("(c p) n -> p c n", p=128), writes=[bstg[k][2]])

    def load_x(ex):
        i = ex % 2
        dma("sp", xgt[i].rearrange("p (s d) -> p s d", s=NST), xg[ex * CAP:(ex + 1) * CAP, :].rearrange("(s p) d -> p s d", p=128),
            reads=b_sc, writes=[bxgt[i]])

    def casts_gu(ex):
        i = ex % 2
        cp("act", wgb[i][:, 0:2048], stage[i][0][:, 0:2048], [bstg[i][0]], [bwg[i]])
        cp("dve", wgb[i][:, 2048:4096], stage[i][0][:, 2048:4096], [bstg[i][0]], [bwg[i]])
        cp("dve", wub[i][:, 0:2048], stage[i][1][:, 0:2048], [bstg[i][1]], [bwu[i]])
        cp("act", wub[i][:, 2048:4096], stage[i][1][:, 2048:4096], [bstg[i][1]], [bwu[i]])

    def cast_d(ex):
        i = ex % 2
        cp("pool", wdb[i][:, 0:2560], stage[i][2][:, 0:2560], [bstg[i][2]], [bwd[i]])
        cp("act", wdb[i][:, 2560:3328], stage[i][2][:, 2560:3328], [bstg[i][2]], [bwd[i]])
        cp("dve", wdb[i][:, 3328:4096], stage[i][2][:, 3328:4096], [bstg[i][2]], [bwd[i]])

    loads(0); load_x(0); loads(1); load_x(1); casts_gu(0); cast_d(0)
    for ex in range(NE):
        i = ex % 2
        XT3 = XT[i].rearrange("p (c s) -> p c s", c=8)
        for s4 in range(NST):
            pb = rot("xtr", [0, 1])
            for c in range(8):
                tr(PS(pb, BF16)[:, c * 128:(c + 1) * 128], xgt[i][:, s4 * D + c * 128:s4 * D + (c + 1) * 128], ident_bf,
                   [bxgt[i], bC], [pbuf[pb]])
            cp("act" if s4 % 2 else "dve", XT3[:, :, s4 * 128:(s4 + 1) * 128], PS(pb, BF16).rearrange("p (c t) -> p c t", c=8),
               [pbuf[pb]], [bXT[i]])
        if ex + 2 < NE:
            load_x(ex + 2)
        wg3 = wgb[i].rearrange("p (c n) -> p c n", c=8)
        wu3 = wub[i].rearrange("p (c n) -> p c n", c=8)
        wd3 = wdb[i].rearrange("p (c n) -> p c n", c=4)
        for fc in range(4):
            pG = rot("G", [2, 3])
            pUp = rot("Up", [4, 5])
            for c in range(8):
                mm(PS(pG)[:, 0:CAP], wg3[:, c, fc * 128:(fc + 1) * 128], XT3[:, c, :], c == 0, c == 7, [bwg[i], bXT[i]], [pbuf[pG]])
            for c in range(8):
                mm(PS(pUp)[:, 0:CAP], wu3[:, c, fc * 128:(fc + 1) * 128], XT3[:, c, :], c == 0, c == 7, [bwu[i], bXT[i]], [pbuf[pUp]])
            isg = rot("sg", [0, 1])
            act(sg[isg], PS(pG)[:, 0:CAP], AF.Silu, [pbuf[pG]], [bsg[isg]])
            tt("dve", hidT[i][:, fc * CAP:(fc + 1) * CAP], sg[isg], PS(pUp)[:, 0:CAP], ALU.mult, [bsg[isg], pbuf[pUp]], [bhid[i]])
        for s4 in range(NST):
            iy = rot("yt", [0, 1, 2])
            for half in range(2):
                pY = rot("Y", [6, 7])
                for fc in range(4):
                    mm(PS(pY), hidT[i][:, fc * CAP + s4 * 128:fc * CAP + (s4 + 1) * 128], wd3[:, fc, half * 512:(half + 1) * 512],
                       fc == 0, fc == 3, [bhid[i], bwd[i]], [pbuf[pY]])
                cp("act" if half else "dve", yt[iy][:, half * 512:(half + 1) * 512], PS(pY),
                   [pbuf[pY]], [byt[iy]])
            r0 = ex * CAP + s4 * 128
            b_ys.append(P.buf())
            dma("sp", ybuf[r0:r0 + 128, :], yt[iy], reads=[byt[iy]], writes=[b_ys[-1]])
        if ex + 1 < NE:
            cast_d(ex + 1)
            casts_gu(ex + 1)
        if ex + 2 < NE:
            loads(ex + 2)

    P.barrier()
    A.top = top_route
    NB = 4
    gfbc = A.alloc(D); bgf = P.buf()
    dma("sp", gfbc, gf_d, writes=[bgf])
    x1c = [A.alloc(D) for _ in range(NB)]; bx1c = [P.buf() for _ in range(NB)]
    Y1 = [A.alloc(D, BF16) for _ in range(NB)]; bY1 = [P.buf() for _ in range(NB)]
    Y2 = [A.alloc(D, BF16) for _ in range(NB)]; bY2 = [P.buf() for _ in range(NB)]
    oo = [A.alloc(D) for _ in range(NB)]; boo = [P.buf() for _ in range(NB)]
    jk2 = A.alloc(D, BF16); bjk2 = P.buf()
    smf = [A.alloc(2) for _ in range(NB)]; bsmf = [P.buf() for _ in range(NB)]

    def cload(gt):
        i = gt % NB
        dma("sp", x1c[i], x1s[gt * 128:(gt + 1) * 128, :], reads=[b_x1s[gt]], writes=[bx1c[i]])
        for (Yk, bYk, dki) in ((Y1, bY1, d1i), (Y2, bY2, d2i)):
            P.op("pool", lambda e, o_=Yk[i], x_=dki[:, gt:gt + 1]: e.indirect_dma_start(
                out=o_, out_offset=None, in_=ybuf, in_offset=bass.IndirectOffsetOnAxis(ap=x_, axis=0)),
                [b_ybuf, bR] + b_ys, [bYk[i]], dma=True)

    for gt in range(NB - 1):
        cload(gt)
    for gt in range(NT):
        i = gt % NB
        if gt + NB - 1 < NT:
            cload(gt + NB - 1)
        stt("dve", x1c[i], Y1[i], gate1[:, gt:gt + 1], x1c[i], ALU.mult, ALU.add, [bY1[i], bR, bx1c[i]], [bx1c[i]])
        stt("dve", x1c[i], Y2[i], gate2[:, gt:gt + 1], x1c[i], ALU.mult, ALU.add, [bY2[i], bR, bx1c[i]], [bx1c[i]])
        act(jk2, x1c[i], AF.Square, [bx1c[i]], [bjk2, bsmf[i]], accum=smf[i][:, 0:1])
        rsqrt_mean(smf[i][:, 0:1], smf[i][:, 0:1], D, [bsmf[i], bC], [bsmf[i]])
        act(oo[i], x1c[i], AF.Copy, [bx1c[i], bsmf[i]], [boo[i]], scale=smf[i][:, 0:1])
        tt("pool" if gt % 3 else "dve", oo[i], oo[i], gfbc, ALU.mult, [boo[i], bgf], [boo[i]])
        dma("sp", out_d[gt * 128:(gt + 1) * 128, :], oo[i], reads=[boo[i]], writes=[b_out])
    P.final_wait("sp")
    P.emit(st)
    st.close()
    return nc, P


_CACHE = {}


def kernel(x, norm1_g, w_in, attn_norm_g, hgrn_gamma, hgrn_norm_g, w_out, norm2_g,
           w_group, b_group, w_router, b_router, w_gate, w_up, w_down, norm_f_g):
    f32 = np.float32
    x = np.asarray(x, f32)
    if "nc" not in _CACHE:
        _CACHE["nc"] = build()
    nc, _ = _CACHE["nc"]
    cons = host_consts()

    def bc128(v):
        return np.ascontiguousarray(np.broadcast_to(np.asarray(v, f32).reshape(1, -1), (128, v.size)))

    def fm(v, c):
        return np.ascontiguousarray(np.asarray(v, f32).reshape(c, 128).T)

    wr = np.concatenate([np.asarray(w_group, f32)[0], np.asarray(w_router, f32)[0]], axis=1)
    br = np.concatenate([np.asarray(b_group, f32)[0], np.asarray(b_router, f32)[0]], axis=0)
    shared = {
        "w_in": np.ascontiguousarray(np.asarray(w_in, f32)[0]),
        "w_out": np.ascontiguousarray(np.asarray(w_out, f32)[0]),
        "g1bc": bc128(np.asarray(norm1_g, f32)[0]),
        "g2bc": bc128(np.asarray(norm2_g, f32)[0]),
        "gfbc": bc128(np.asarray(norm_f_g, f32)),
        "ga": fm(np.asarray(attn_norm_g, f32)[0], 4),
        "gh": fm(np.asarray(hgrn_norm_g, f32)[0], 4),
        "gam": np.ascontiguousarray(np.concatenate([fm(np.asarray(hgrn_gamma, f32)[0], 4),
                                                    fm(np.asarray(hgrn_gamma, f32)[1], 4)], axis=1)),
        "wr": np.ascontiguousarray(wr.reshape(8, 128, 36).transpose(1, 0, 2)),
        "brbc": bc128(br),
        "w_gate": np.ascontiguousarray(np.asarray(w_gate, f32)[0]),
        "w_up": np.ascontiguousarray(np.asarray(w_up, f32)[0]),
        "w_down": np.ascontiguousarray(np.asarray(w_down, f32)[0]),
    }
    for k, v in cons.items():
        shared["c_" + k] = v
    xs = x.reshape(NCORES, T, D)
    in_maps = [dict(shared, x=np.ascontiguousarray(xs[i])) for i in range(NCORES)]
    res = run_bass_kernel_spmd(nc, in_maps, core_ids=list(range(NCORES)))
    out = np.stack([np.asarray(r["out"], f32).reshape(T, D) for r in res.results], axis=0)
    return out.reshape(16, S, D)
```

```python
import numpy as np
import ml_dtypes
from contextlib import ExitStack
import concourse.bass as bass
import concourse.mybir as mybir
from concourse.bass_utils import run_bass_kernel_spmd

F32 = mybir.dt.float32
BF16 = mybir.dt.bfloat16
I32 = mybir.dt.int32
AF = mybir.ActivationFunctionType
ALU = mybir.AluOpType
AX = mybir.AxisListType

COMPUTE = ("pe", "act", "dve", "pool")
QUEUES = ("sp", "act", "pool")
KSLOTS = {"sp": 12, "act": 8, "pool": 8}
ENGS = ("pe", "act", "dve", "pool", "sp")


class Buf:
    __slots__ = ("name", "lw", "rd")

    def __init__(self, name):
        self.name = name
        self.lw = None
        self.rd = []


class Op:
    __slots__ = ("eng", "fn", "tl", "ord", "waits", "clock", "is_dma", "idx")


class Prog:
    def __init__(self, nc, same_engine_sync=("act", "dve", "pool")):
        self.nc = nc
        self.ops = []
        self.eng_ops = {e: [] for e in ENGS}
        self.known = {e: {} for e in ENGS}
        self.cnt = {e: 0 for e in COMPUTE}
        self.dcnt = {q: 0 for q in QUEUES}
        self.last = {}
        self.same_sync = set(same_engine_sync)
        self.nbuf = 0

    def buf(self, name=None):
        self.nbuf += 1
        return Buf(name or f"b{self.nbuf}")

    def _need(self, X, tl, o, clock, waits):
        kn = self.known[X]
        if kn.get(tl, 0) >= o:
            return
        waits.append((tl, o))
        for k, v in clock.items():
            if kn.get(k, 0) < v:
                kn[k] = v

    def op(self, eng, fn, reads=(), writes=(), dma=False):
        o = Op()
        o.eng, o.fn, o.is_dma, o.idx = eng, fn, dma, len(self.ops)
        deps = set()
        for b in reads:
            if b.lw is not None:
                deps.add(b.lw)
        for b in writes:
            if b.lw is not None:
                deps.add(b.lw)
            deps.update(b.rd)
        waits = []
        if dma:
            n = self.dcnt[eng]
            self.dcnt[eng] = n + 1
            K = KSLOTS[eng]
            o.tl = ("q", eng, n % K)
            o.ord = n // K + 1
            if o.ord > 1:
                po, pc = self.last[o.tl]
                self._need(eng, o.tl, po, pc, waits)
        else:
            self.cnt[eng] += 1
            o.tl = eng
            o.ord = self.cnt[eng]
        for j in sorted(deps, reverse=True):
            d = self.ops[j]
            if d.tl == eng and not dma and eng not in self.same_sync:
                continue
            self._need(eng, d.tl, d.ord, d.clock, waits)
        o.waits = waits
        ck = dict(self.known[eng])
        ck[o.tl] = o.ord
        o.clock = ck
        self.last[o.tl] = (o.ord, ck)
        for b in reads:
            b.rd.append(o.idx)
        for b in writes:
            b.lw = o.idx
            b.rd = []
        self.ops.append(o)
        self.eng_ops[eng].append(o)
        return o

    def _sync_all(self, engines, skip_bg=False):
        lasts = dict(self.last)
        if skip_bg:
            lasts = {tl: v for tl, v in lasts.items() if not (isinstance(tl, tuple) and tl[1] == "act")}
        for e in engines:
            o = Op()
            o.eng, o.fn, o.is_dma, o.idx = e, None, False, len(self.ops)
            waits = []
            for tl, (od, ck) in lasts.items():
                self._need(e, tl, od, ck, waits)
            o.waits = waits
            o.tl, o.ord = None, 0
            o.clock = dict(self.known[e])
            self.ops.append(o)
            self.eng_ops[e].append(o)

    def barrier(self):
        self._sync_all(ENGS, skip_bg=True)

    def final_wait(self, eng="sp"):
        self._sync_all((eng,))

    def emit(self, stack):
        nc = self.nc
        waited = {e: set() for e in COMPUTE}
        for o in self.ops:
            for tl, od in o.waits:
                if isinstance(tl, str):
                    waited[tl].add(od)
        rank = {e: {od: i + 1 for i, od in enumerate(sorted(waited[e]))} for e in COMPUTE}
        sems = {}
        for e in COMPUTE:
            sems[e] = stack.enter_context(nc.semaphore(f"s_{e}"))
        for q in QUEUES:
            for k in range(KSLOTS[q]):
                sems[("q", q, k)] = stack.enter_context(nc.semaphore(f"s_{q}{k}"))
        self.stats = {e: [len(self.eng_ops[e]), 0] for e in self.eng_ops}
        block = stack.enter_context(nc.Block())
        engmap = {"pe": "tensor", "act": "scalar", "dve": "vector", "pool": "gpsimd", "sp": "sync"}

        def make(e):
            def body(engine):
                for o in self.eng_ops[e]:
                    for tl, od in o.waits:
                        if isinstance(tl, str):
                            engine.wait_ge(sems[tl], rank[tl][od])
                        else:
                            engine.wait_ge(sems[tl], 16 * od)
                        self.stats[e][1] += 1
                    if o.fn is None:
                        continue
                    ins = o.fn(engine)
                    if o.is_dma:
                        ins.then_inc(sems[o.tl], 16)
                    elif o.ord in rank[o.tl]:
                        ins.then_inc(sems[o.tl], 1)
            return body

        for e in ENGS:
            getattr(block, engmap[e])(make(e))


class Arena:
    def __init__(self, ap, words):
        self.ap = ap
        self.words = words
        self.top = 0

    def alloc(self, n_elems, dtype=F32):
        bpe = 4 if dtype in (F32, I32) else 2
        w = (n_elems * bpe + 3) // 4
        w = (w + 15) // 16 * 16
        assert self.top + w <= self.words, f"arena overflow {self.top}+{w}>{self.words}"
        v = self.ap[:, self.top:self.top + w]
        self.top += w
        if dtype != F32:
            v = v.bitcast(dtype)
        return v[:, 0:n_elems]


NCORES = 8
S = 2048
NSEQ = 2
T = NSEQ * S
NT = T // 128
D = 1024
NE = 32
CAP = 384
NSLOT = NE * CAP
TRASH = NSLOT
EPS = 1e-6
DILS = (1, 4, 16)
NEG = -30000.0


def tokslice(c, vb):
    dil = DILS[c]
    if c == 0:
        return slice(128 * vb, 128 * vb + 128, 1)
    if c == 1:
        r, n = vb // 4, vb % 4
        st = 4 * 128 * n + r
        return slice(st, st + 4 * 127 + 1, 4)
    return slice(vb, vb + 16 * 127 + 1, 16)


def prev_vb(c, vb):
    if c == 0:
        return vb - 1 if vb > 0 else None
    if c == 1:
        return vb - 1 if vb % 4 > 0 else None
    return None


def host_consts():
    bf = ml_dtypes.bfloat16
    k = np.arange(128)[:, None].astype(np.float64)
    q = np.arange(128)[None, :].astype(np.float64)
    bt = np.zeros((128, 8, 3, 2, 128), np.float32)
    for h in range(8):
        s = 2.0 ** (-(h + 1))
        for c, dil in enumerate(DILS):
            dg = np.where(q >= k, -s * dil * (q - k), NEG)
            of = np.where(k >= q, -s * dil * (q + 128 - k), NEG)
            bt[:, h, c, 0, :] = dg
            bt[:, h, c, 1, :] = of
    cons = {}
    cons["btab"] = bt.reshape(128, 48 * 128).astype(bf)
    cons["ident_bf"] = np.eye(128, dtype=np.float32).astype(bf)
    cons["ident32"] = np.eye(128, dtype=np.float32)
    cm = (k <= q).astype(np.float32)
    cons["cmask"] = np.concatenate([cm, cm], axis=1).astype(np.float32)
    sm = np.ones((128, 512), np.float32)
    sm[:, 0::128] = 0.0
    cons["scanmask"] = sm
    bd = np.zeros((128, 128), np.float32)
    bd[0:64, 0:64] = 1.0
    bd[64:128, 64:128] = 1.0
    cons["bd_bf"] = bd.astype(bf)
    cons["ones_bf"] = np.ones((128, 128), np.float32).astype(bf)
    cons["tri_bf"] = (k < q).astype(np.float32).astype(bf)
    cons["ebase"] = np.broadcast_to((np.arange(NE) * CAP).astype(np.float32)[None, :], (128, NE)).copy()
    cons["zeros_bf"] = np.zeros((NSLOT + 128, D), bf)
    cons["zeros32"] = np.zeros((128, D), np.float32)
    return cons


def build():
    nc = bass.Bass("TRN2", target_bir_lowering=False)
    P = Prog(nc)
    st = ExitStack()

    def din(name, shape, dt=F32):
        return nc.dram_tensor(name, list(shape), dt, kind="ExternalInput").ap()

    x_d = din("x", [T, D])
    win_d = din("w_in", [D, 3584])
    wout_d = din("w_out", [D, D])
    g1_d = din("g1bc", [128, D])
    g2_d = din("g2bc", [128, D])
    gf_d = din("gfbc", [128, D])
    ga_d = din("ga", [128, 4])
    gh_d = din("gh", [128, 4])
    gam_d = din("gam", [128, 8])
    wr_d = din("wr", [128, 8, 36])
    br_d = din("brbc", [128, 36])
    wg_d = din("w_gate", [NE, D, 512])
    wu_d = din("w_up", [NE, D, 512])
    wd_d = din("w_down", [NE, 512, D])
    cshape = {"btab": ([128, 48 * 128], BF16), "ident_bf": ([128, 128], BF16), "ident32": ([128, 128], F32),
              "cmask": ([128, 256], F32), "scanmask": ([128, 512], F32), "bd_bf": ([128, 128], BF16),
              "ones_bf": ([128, 128], BF16), "tri_bf": ([128, 128], BF16), "ebase": ([128, NE], F32),
              "zeros_bf": ([NSLOT + 128, D], BF16), "zeros32": ([128, D], F32)}
    cd = {k: din("c_" + k, v[0], v[1]) for k, v in cshape.items()}
    out_d = nc.dram_tensor("out", [T, D], F32, kind="ExternalOutput").ap()
    x1s = nc.dram_tensor("x1s", [T, D], F32, kind="Internal").ap()
    h2s = nc.dram_tensor("h2s", [T, D], BF16, kind="Internal").ap()
    xg = nc.dram_tensor("xg", [NSLOT + 128, D], BF16, kind="Internal").ap()
    ybuf = nc.dram_tensor("ybuf", [NSLOT + 128, D], BF16, kind="Internal").ap()
    b_x1s = [P.buf() for _ in range(NT)]
    b_h2s = [P.buf() for _ in range(NT)]
    vsd = [nc.dram_tensor(f"vsd{i}", [S, 128], BF16, kind="Internal").ap() for i in range(2)]
    b_vs = [P.buf() for _ in range(2)]
    b_xg = P.buf("xg")
    b_xgz = [P.buf() for _ in range(4)]
    b_xge = [P.buf() for _ in range(NE)]
    b_ybuf = P.buf("ybuf")
    b_out = P.buf("out")

    AW = 53000
    arena_t = st.enter_context(nc.sbuf_tensor("arena", [128, AW], F32))
    A = Arena(arena_t[:, :], AW)
    pbank = [st.enter_context(nc.psum_tensor(f"pb{i}", [128, 512], F32)) for i in range(8)]
    pbuf = [P.buf(f"pb{i}") for i in range(8)]

    def PS(i, dt=F32):
        v = pbank[i][:, :]
        return v.bitcast(BF16) if dt == BF16 else v

    rot_state = {}

    def rot(name, banks):
        i = rot_state.get(name, 0)
        rot_state[name] = i + 1
        return banks[i % len(banks)]

    def dma(q, out, in_, reads=(), writes=()):
        P.op(q, lambda e: e.dma_start(out=out, in_=in_), reads, writes, dma=True)

    def mm(out, lhsT, rhs, start, stop, reads, writes):
        P.op("pe", lambda e: e.matmul(out, lhsT=lhsT, rhs=rhs, start=start, stop=stop), reads, writes)

    def tr(out, in_, ident, reads, writes):
        P.op("pe", lambda e: e.transpose(out=out, in_=in_, identity=ident), reads, writes)

    def act(out, in_, func, reads, writes, bias=None, scale=None, accum=None):
        kw = {}
        if bias is not None:
            kw["bias"] = bias
        if scale is not None:
            kw["scale"] = scale
        if accum is not None:
            kw["accum_out"] = accum
        P.op("act", lambda e: e.activation(out=out, in_=in_, func=func, **kw), reads, writes)

    def tt(eng, out, in0, in1, op, reads, writes):
        P.op(eng, lambda e: e.tensor_tensor(out=out, in0=in0, in1=in1, op=op), reads, writes)

    def ts(eng, out, in0, s1, s2, op0, op1, reads, writes):
        if s2 is None:
            P.op(eng, lambda e: e.tensor_scalar(out=out, in0=in0, scalar1=s1, scalar2=None, op0=op0), reads, writes)
        else:
            P.op(eng, lambda e: e.tensor_scalar(out=out, in0=in0, scalar1=s1, scalar2=s2, op0=op0, op1=op1), reads, writes)

    def stt(eng, out, in0, scalar, in1, op0, op1, reads, writes):
        P.op(eng, lambda e: e.scalar_tensor_tensor(out=out, in0=in0, scalar=scalar, in1=in1, op0=op0, op1=op1),
             reads, writes)

    def cp(eng, out, in_, reads, writes):
        if eng == "act":
            act(out, in_, AF.Copy, reads, writes)
        else:
            P.op(eng, lambda e: e.tensor_copy(out=out, in_=in_), reads, writes)

    def rsqrt_mean(out, in_, n, reads, writes):
        act(out, in_, AF.Ln, reads, writes, bias=epsc[:, 0:1], scale=1.0 / n)
        act(out, out, AF.Exp, writes, writes, scale=-0.5)

    C = {}
    bC = P.buf("consts")
    for k, (shp, dt) in cshape.items():
        if k in ("zeros_bf", "btab", "zeros32"):
            continue
        C[k] = A.alloc(shp[1], dt)
        dma("sp", C[k], cd[k], writes=[bC])
    epsc = A.alloc(1)
    P.op("pool", lambda e: e.memset(epsc, EPS), writes=[bC])
    g1bc = A.alloc(D); dma("sp", g1bc, g1_d, writes=[bC])
    ga = A.alloc(4); dma("sp", ga, ga_d, writes=[bC])
    gh = A.alloc(4); dma("sp", gh, gh_d, writes=[bC])
    gam = A.alloc(8); dma("sp", gam, gam_d, writes=[bC])
    lb = A.alloc(4); oml = A.alloc(4); noml = A.alloc(4)
    tt("dve", lb, gam[:, 0:4], gam[:, 4:8], ALU.subtract, [bC], [bC])
    act(lb, lb, AF.Sigmoid, [bC], [bC])
    ts("dve", oml, lb, -1.0, 1.0, ALU.mult, ALU.add, [bC], [bC])
    ts("dve", noml, oml, -1.0, None, ALU.mult, None, [bC], [bC])
    Lall = A.alloc(NT * 36)
    bL = P.buf("Lall")
    top_persist = A.top

    win = A.alloc(8 * 3584, BF16); bwin = P.buf("win")
    win3 = win.rearrange("p (c n) -> p c n", c=8)
    g2bc = A.alloc(D); dma("sp", g2bc, g2_d, writes=[bC])
    wr = A.alloc(8 * 36); dma("sp", wr, wr_d.rearrange("p c n -> p (c n)"), writes=[bC])
    wr3 = wr.rearrange("p (c n) -> p c n", c=8)
    brbc = A.alloc(36); dma("sp", brbc, br_d, writes=[bC])
    bwin_h = P.buf("win_h")
    for c in range(8):
        dma("pool", win3[:, c, 0:1536], win_d[c * 128:(c + 1) * 128, 0:1536], writes=[bwin])
    for c in range(8):
        dma("pool", win3[:, c, 1536:3584], win_d[c * 128:(c + 1) * 128, 1536:3584], writes=[bwin_h])
    hT = A.alloc(8 * S, BF16); bhT = P.buf("hT")
    hT3 = hT.rearrange("p (c t) -> p c t", c=8)
    mixT = A.alloc(8 * S, BF16)
    mixT3 = mixT.rearrange("p (c t) -> p c t", c=8)
    bmix = [P.buf(f"mix{i}") for i in range(8)]
    scratch = A.top

    ident_bf, ident32 = C["ident_bf"], C["ident32"]
    ones_bf = C["ones_bf"]

    for sq in range(NSEQ):
        if sq > 0:
            P.barrier()
        A.top = scratch
        NX = 4
        xt = [A.alloc(D) for _ in range(NX)]; bxt = [P.buf() for _ in range(NX)]
        hb = [A.alloc(D, BF16) for _ in range(NX)]; bhb = [P.buf() for _ in range(NX)]
        sm1 = [A.alloc(2) for _ in range(NX)]; bsm1 = [P.buf() for _ in range(NX)]
        junk = A.alloc(D, BF16); bjunk = P.buf("junk")
        for t in range(NX - 1):
            dma("sp", xt[t], x_d[(sq * 16 + t) * 128:(sq * 16 + t + 1) * 128, :], writes=[bxt[t]])
        for t in range(16):
            gt = sq * 16 + t
            i = t % NX
            if t + NX - 1 < 16:
                t2 = t + NX - 1
                dma("sp", xt[t2 % NX], x_d[(sq * 16 + t2) * 128:(sq * 16 + t2 + 1) * 128, :], writes=[bxt[t2 % NX]])
            act(junk, xt[i], AF.Square, [bxt[i]], [bjunk, bsm1[i]], accum=sm1[i][:, 0:1])
            rsqrt_mean(sm1[i][:, 0:1], sm1[i][:, 0:1], D, [bsm1[i], bC], [bsm1[i]])
            stt("dve", hb[i], xt[i], sm1[i][:, 0:1], g1bc, ALU.mult, ALU.mult, [bxt[i], bsm1[i], bC], [bhb[i]])
            pb = rot("tr", [0, 1])
            for c in range(8):
                tr(PS(pb, BF16)[:, c * 128:(c + 1) * 128], hb[i][:, c * 128:(c + 1) * 128], ident_bf,
                   [bhb[i], bC], [pbuf[pb]])
            cp("act" if t % 2 else "dve", hT3[:, :, t * 128:(t + 1) * 128],
               PS(pb, BF16).rearrange("p (c t) -> p c t", c=8), [pbuf[pb]], [bhT])

        def proj_fm(col0, cb):
            pb = rot("proj", [0, 1])
            for c in range(8):
                mm(PS(pb), win3[:, c, col0:col0 + 128], hT3[:, c, cb * 512:(cb + 1) * 512], c == 0, c == 7,
                   [bwin if col0 < 1536 else bwin_h, bhT], [pbuf[pb]])
            return pb

        P.barrier()
        A.top = scratch
        QT = A.alloc(S, BF16); KT = A.alloc(S, BF16); bQT = P.buf(); bKT = P.buf()
        VcS = [[A.alloc(16 * 128, BF16) for _ in range(3)] for _ in range(2)]
        bVcS = [[P.buf() for _ in range(3)] for _ in range(2)]
        Uacc = A.alloc(S); Zacc = A.alloc(S); bUacc = P.buf(); bZacc = P.buf()
        ssacc = A.alloc(S); bss = P.buf()
        PT = [A.alloc(512, BF16) for _ in range(3)]; bPT = [P.buf() for _ in range(3)]
        osq = [A.alloc(512, BF16) for _ in range(2)]; bosq = [P.buf() for _ in range(2)]
        btab = A.alloc(12 * 128, BF16); bbt = P.buf()
        btab3 = btab.rearrange("p (j q) -> p j q", q=128)
        def vproj_hp(hp_):
            k_ = hp_ % 2
            Vn = VcS[k_][0]
            for g in range(4):
                pb = rot("proj", [0, 1])
                for j in range(4):
                    tl = g * 4 + j
                    for ch in range(8):
                        mm(PS(pb)[:, j * 128:(j + 1) * 128], hT3[:, ch, tl * 128:(tl + 1) * 128],
                           win3[:, ch, 1024 + hp_ * 128:1024 + (hp_ + 1) * 128], ch == 0, ch == 7,
                           [bwin, bhT], [pbuf[pb]])
                cp("act" if g % 2 else "dve", Vn[:, g * 512:(g + 1) * 512], PS(pb), [pbuf[pb]], [bVcS[k_][0]])
            dma("sp", vsd[k_].rearrange("(v p) f -> p v f", p=128), Vn.rearrange("p (v f) -> p v f", f=128),
                reads=[bVcS[k_][0]], writes=[b_vs[k_]])
            for r in range(4):
                dma("sp", VcS[k_][1].rearrange("p (r n f) -> p r n f", r=4, n=4)[:, r, :, :],
                    vsd[k_].rearrange("(n p r) f -> p r n f", n=4, p=128, r=4)[:, r, :, :],
                    reads=[b_vs[k_]], writes=[bVcS[k_][1]])
            dma("sp", VcS[k_][2].rearrange("p (r f) -> p r f", f=128), vsd[k_].rearrange("(p r) f -> p r f", r=16),
                reads=[b_vs[k_]], writes=[bVcS[k_][2]])

        vproj_hp(0)
        for hp in range(4):
            Vc, bVc = VcS[hp % 2], bVcS[hp % 2]
            dma("sp", btab, cd["btab"][:, hp * 1536:(hp + 1) * 1536], writes=[bbt])
            for cb in range(4):
                pb = proj_fm(hp * 128, cb)
                ts("dve", QT[:, cb * 512:(cb + 1) * 512], PS(pb), 0.125, None, ALU.mult, None, [pbuf[pb]], [bQT])
                pb = proj_fm(512 + hp * 128, cb)
                cp("act", KT[:, cb * 512:(cb + 1) * 512], PS(pb), [pbuf[pb]], [bKT])
            if hp + 1 < 4:
                vproj_hp(hp + 1)
            def att_scores(c, vb):
                pv = prev_vb(c, vb)
                kbs = [vb] + ([pv] if pv is not None else [])
                qs = tokslice(c, vb)
                pS = rot("S", [2, 3])
                ip = rot("PT", [0, 1, 2])
                for ty, kb in enumerate(kbs):
                    ks = tokslice(c, kb)
                    for e in range(2):
                        reg = PS(pS)[:, (ty * 2 + e) * 128:(ty * 2 + e + 1) * 128]
                        mm(reg, KT[64 * e:64 * e + 64, ks], QT[64 * e:64 * e + 64, qs], True, False,
                           [bKT, bQT], [pbuf[pS]])
                        mm(reg, ident_bf, btab3[:, (e * 3 + c) * 2 + ty, :], False, True, [bC, bbt], [pbuf[pS]])
                n = 256 * len(kbs)
                act(PT[ip][:, 0:n], PS(pS)[:, 0:n], AF.Exp, [pbuf[pS]], [bPT[ip]])
                return ip, kbs

            def att_pv(c, g, j, pU, pZ, ip, kbs):
                Vc3 = Vc[c].rearrange("p (v f) -> p v f", f=128)
                for e in range(2):
                    for ty, kb in enumerate(kbs):
                        mm(PS(pU)[64 * e:64 * e + 64, j * 128:(j + 1) * 128], Vc3[:, kb, 64 * e:64 * e + 64],
                           PT[ip][:, (ty * 2 + e) * 128:(ty * 2 + e + 1) * 128], ty == 0, ty == len(kbs) - 1,
                           [bVc[c], bPT[ip]], [pbuf[pU]])
                    for ty, kb in enumerate(kbs):
                        mm(PS(pZ)[64 * e:64 * e + 64, j * 128:(j + 1) * 128], ones_bf[:, 0:64],
                           PT[ip][:, (ty * 2 + e) * 128:(ty * 2 + e + 1) * 128], ty == 0, ty == len(kbs) - 1,
                           [bC, bPT[ip]], [pbuf[pZ]])
                if j == 3:
                    for (acc, bacc, pb_) in ((Uacc, bUacc, pU), (Zacc, bZacc, pZ)):
                        if c == 0:
                            dst = acc[:, g * 512:(g + 1) * 512]
                            src = PS(pb_)
                        elif c == 1:
                            dst = acc[:, g:g + 4 * 511 + 1:4]
                            src = PS(pb_)
                        else:
                            dst = acc.rearrange("a (p r) -> a r p", r=16)[:, 4 * g:4 * g + 4, :]
                            src = PS(pb_).rearrange("a (j p) -> a j p", j=4)
                        if c == 0:
                            cp("dve", dst, src, [pbuf[pb_]], [bacc])
                        else:
                            tt("dve", dst, dst, src, ALU.add, [pbuf[pb_], bacc], [bacc])

            pend = None
            for c in range(3):
                for g in range(4):
                    pU = rot("U", [4, 5])
                    pZ = rot("Z", [6, 7])
                    for j in range(4):
                        ip, kbs = att_scores(c, g * 4 + j)
                        if pend is not None:
                            att_pv(*pend)
                        pend = (c, g, j, pU, pZ, ip, kbs)
            att_pv(*pend)
            act(Zacc, Zacc, AF.Ln, [bZacc], [bZacc])
            act(Zacc, Zacc, AF.Exp, [bZacc], [bZacc], scale=-1.0)
            tt("dve", Uacc, Uacc, Zacc, ALU.mult, [bUacc, bZacc], [bUacc])
            cp("pool", mixT3[:, hp, :], Uacc, [bUacc], [bmix[hp]])
            for cb in range(4):
                io = rot("osq", [0, 1])
                act(osq[io], Uacc[:, cb * 512:(cb + 1) * 512], AF.Square, [bUacc], [bosq[io]])
                pb = rot("proj", [0, 1])
                mm(PS(pb), ones_bf, osq[io], True, True, [bC, bosq[io]], [pbuf[pb]])
                if hp == 0:
                    cp("dve", ssacc[:, cb * 512:(cb + 1) * 512], PS(pb), [pbuf[pb]], [bss])
                else:
                    tt("dve", ssacc[:, cb * 512:(cb + 1) * 512], ssacc[:, cb * 512:(cb + 1) * 512], PS(pb), ALU.add,
                       [pbuf[pb], bss], [bss])
        rsqrt_mean(ssacc, ssacc, 512, [bss, bC], [bss])
        for hp in range(4):
            stt("dve", mixT3[:, hp, :], mixT3[:, hp, :], ga[:, hp:hp + 1], ssacc, ALU.mult, ALU.mult,
                [bmix[hp], bss, bC], [bmix[hp]])

        P.barrier()
        A.top = scratch
        qinT = A.alloc(S, BF16); kinT = A.alloc(S, BF16); kendT = A.alloc(S, BF16); gsT = A.alloc(S, BF16)
        bqin = P.buf(); bkin = P.buf(); bkend = P.buf(); bgs = P.buf()
        iV = A.alloc(16 * 128, BF16); biV = P.buf()
        dec = A.alloc(16); bdec = P.buf()
        sigf = A.alloc(S); bsigf = [P.buf() for _ in range(2)]
        tBf = A.alloc(S); btB = [P.buf() for _ in range(2)]
        tEf = A.alloc(S); btE = [P.buf() for _ in range(2)]
        tCf = A.alloc(S); btC = [P.buf() for _ in range(2)]
        qsT = A.alloc(S, BF16); bqs = P.buf()
        Am = [A.alloc(256, BF16) for _ in range(2)]; bAmh = [[P.buf(), P.buf()] for _ in range(2)]
        ket = [A.alloc(512, BF16) for _ in range(2)]; bket = [P.buf() for _ in range(2)]
        osq = [A.alloc(512, BF16) for _ in range(2)]; bosq = [P.buf() for _ in range(2)]
        decm = A.alloc(1024); bdecm = P.buf()
        Sall = A.alloc(1024, BF16); bSall = P.buf()
        Sall3 = Sall.rearrange("p (v t) -> p v t", t=16)
        if sq == 0:
            nr = (NSLOT + 128) // 4
            for i in range(4):
                dma("act", xg[i * nr:(i + 1) * nr, :], cd["zeros_bf"][i * nr:(i + 1) * nr, :], writes=[b_xgz[i]])
            dma("act", ybuf[NSLOT:NSLOT + 128, :], cd["zeros_bf"][0:128, :], writes=[b_ybuf])
        for hp in range(4):
            cq, cf, ci, cg = 1536 + hp * 128, 2048 + hp * 128, 2560 + hp * 128, 3072 + hp * 128
            for cb in range(4):
                cs = slice(cb * 512, (cb + 1) * 512)
                pf = proj_fm(cf, cb)
                act(sigf[:, cs], PS(pf), AF.Sigmoid, [pbuf[pf]], [bsigf[cb // 2]])
            for cb in range(4):
                cs = slice(cb * 512, (cb + 1) * 512)
                pq = proj_fm(cq, cb)
                act(qsT[:, cs], PS(pq), AF.Silu, [pbuf[pq]], [bqs])
                pg = proj_fm(cg, cb)
                act(gsT[:, cs], PS(pg), AF.Silu, [pbuf[pg]], [bgs])
            HS = [slice(0, 1024), slice(1024, 2048)]
            for h in range(2):
                ts("dve", tBf[:, HS[h]], sigf[:, HS[h]], oml[:, hp:hp + 1], lb[:, hp:hp + 1], ALU.mult, ALU.add,
                   [bsigf[h], bC], [btB[h]])
            for h in range(2):
                act(tBf[:, HS[h]], tBf[:, HS[h]], AF.Ln, [btB[h]], [btB[h]])
            for h in range(2):
                ts("dve", sigf[:, HS[h]], sigf[:, HS[h]], noml[:, hp:hp + 1], oml[:, hp:hp + 1], ALU.mult, ALU.add,
                   [bsigf[h], bC], [bsigf[h]])
            for h in range(2):
                for j in range(2):
                    c5 = slice(h * 1024 + j * 512, h * 1024 + (j + 1) * 512)
                    P.op("dve", lambda e, o_=tEf[:, c5], d_=tBf[:, c5], m_=C["scanmask"]: e.tensor_tensor_scan(
                        out=o_, data0=m_, data1=d_, initial=0.0, op0=ALU.mult, op1=ALU.add), [btB[h], bC], [btE[h]])
            for h in range(2):
                act(tCf[:, HS[h]], tEf[:, HS[h]], AF.Exp, [btE[h]], [btC[h]])
            for h in range(2):
                tt("dve", qinT[:, HS[h]], qsT[:, HS[h]], tCf[:, HS[h]], ALU.mult, [bqs, btC[h]], [bqin])
            for h in range(2):
                act(tCf[:, HS[h]], tEf[:, HS[h]], AF.Exp, [btE[h], btC[h]], [btC[h]], scale=-1.0)
            act(dec[:, 0:16], tEf[:, 127:2048:128], AF.Exp, [btE[0], btE[1]], [bdec])
            for h in range(2):
                tt("dve", kinT[:, HS[h]], sigf[:, HS[h]], tCf[:, HS[h]], ALU.mult, [bsigf[h], btC[h]], [bkin])
            for h in range(2):
                tt("dve", kendT[:, HS[h]].rearrange("p (a b) -> p a b", b=128), kinT[:, HS[h]].rearrange("p (a b) -> p a b", b=128),
                   dec[:, h * 8:(h + 1) * 8].unsqueeze(2).to_broadcast([128, 8, 128]), ALU.mult, [bkin, bdec], [bkend])
            for g in range(4):
                pb = g
                for j in range(4):
                    tl = g * 4 + j
                    for ch in range(8):
                        mm(PS(pb)[:, j * 128:(j + 1) * 128], hT3[:, ch, tl * 128:(tl + 1) * 128], win3[:, ch, ci:ci + 128],
                           ch == 0, ch == 7, [bwin_h, bhT], [pbuf[pb]])
                cp("act" if g % 2 else "dve", iV[:, g * 512:(g + 1) * 512], PS(pb), [pbuf[pb]], [biV])
            iV3 = iV.rearrange("p (v f) -> p v f", f=128)
            cp("pool", decm.rearrange("p (v t) -> p v t", t=16), dec[:, 0:16].unsqueeze(1).to_broadcast([128, 64, 16]),
               [bdec], [bdecm])
            P.op("pool", lambda e: e.memset(decm[:, 0:1024:16], 0.0), [bdecm], [bdecm])
            for g4 in range(4):
                i = g4 % 2
                pt_ = 4 + i
                for j in range(4):
                    tk = g4 * 4 + j
                    tr(PS(pt_, BF16)[:, j * 128:(j + 1) * 128], kendT[:, tk * 128:(tk + 1) * 128], ident_bf, [bkend, bC], [pbuf[pt_]])
                cp("act" if i else "dve", ket[i], PS(pt_, BF16)[:, 0:512], [pbuf[pt_]], [bket[i]])
                for j in range(4):
                    tk = g4 * 4 + j
                    for e in range(2):
                        for vh in range(2):
                            mm(PS(vh)[64 * e:64 * e + 64, tk:512:16], ket[i][:, j * 128 + 64 * e:j * 128 + 64 * e + 64],
                               iV3[:, tk, 64 * e + 32 * vh:64 * e + 32 * vh + 32], True, True, [bket[i], biV], [pbuf[vh]])
            for vh in range(2):
                P.op("dve", lambda e, o_=Sall[:, vh * 512:(vh + 1) * 512], d0=decm[:, vh * 512:(vh + 1) * 512], d1=PS(vh):
                     e.tensor_tensor_scan(out=o_, data0=d0, data1=d1, initial=0.0, op0=ALU.mult, op1=ALU.add),
                     [bdecm, pbuf[vh]], [bSall])

            def stage1(tk):
                i = tk % 2
                cs = slice(tk * 128, (tk + 1) * 128)
                ab_ = 2 if tk % 2 == 0 else 0
                for e in range(2):
                    mm(PS(ab_ + e)[:, 0:128], kinT[64 * e:64 * e + 64, cs], qinT[64 * e:64 * e + 64, cs], True, True,
                       [bkin, bqin], [pbuf[ab_ + e]])
                for e in range(2):
                    tt("dve", Am[i][:, e * 128:(e + 1) * 128], PS(ab_ + e)[:, 0:128], C["cmask"][:, 0:128], ALU.mult,
                       [pbuf[ab_ + e], bC], [bAmh[i][e]])

            def stage2(tk, po):
                i = tk % 2
                cs = slice(tk * 128, (tk + 1) * 128)
                j = tk % 4
                for e in range(2):
                    mm(PS(po)[64 * e:64 * e + 64, j * 128:(j + 1) * 128], iV3[:, tk, 64 * e:64 * e + 64],
                       Am[i][:, e * 128:(e + 1) * 128], True, tk == 0, [biV, bAmh[i][e]], [pbuf[po]])
                    if tk > 0:
                        mm(PS(po)[64 * e:64 * e + 64, j * 128:(j + 1) * 128], Sall3[64 * e:64 * e + 64, :, tk - 1],
                           qinT[64 * e:64 * e + 64, cs], False, True, [bSall, bqin], [pbuf[po]])

            def post_a(cb, po):
                io = cb % 2
                act(osq[io], PS(po), AF.Square, [pbuf[po]], [bosq[io]])

            def post_b(cb, po):
                io = cb % 2
                tmp = tBf[:, cb * 512:(cb + 1) * 512]
                mm(PS(5), C["bd_bf"], osq[io], True, True, [bC, bosq[io]], [pbuf[5]])
                rsqrt_mean(tmp, PS(5), 64, [pbuf[5], bC], [btB[cb // 2]])

            def post_c(cb, po):
                cs = slice(cb * 512, (cb + 1) * 512)
                tmp = tBf[:, cs]
                stt("dve", tmp, PS(po), gh[:, hp:hp + 1], tmp, ALU.mult, ALU.mult, [pbuf[po], bC, btB[cb // 2]], [btB[cb // 2]])
                tt("pool", mixT3[:, 4 + hp, cs], tmp, gsT[:, cs], ALU.mult, [btB[cb // 2], bgs], [bmix[4 + hp]])

            po = None
            pos_ = {}
            stage1(0)
            for tk in range(16):
                if tk + 1 < 16:
                    stage1(tk + 1)
                if tk % 4 == 0:
                    po = rot("O", [6, 7])
                    pos_[tk // 4] = po
                stage2(tk, po)
                g_ = tk // 4
                if tk % 4 == 3:
                    post_a(g_, po)
                if tk % 4 == 0 and g_ >= 1:
                    post_b(g_ - 1, pos_[g_ - 1])
                if tk % 4 == 2 and g_ >= 1:
                    post_c(g_ - 1, pos_[g_ - 1])
            post_b(3, pos_[3])
            post_c(3, pos_[3])

        P.barrier()
        A.top = scratch
        wout = A.alloc(8 * D, BF16); bwout = P.buf("wout")
        wout3 = wout.rearrange("p (c n) -> p c n", c=8)
        dma("pool", wout3, wout_d.rearrange("(c p) n -> p c n", p=128), writes=[bwout])
        xt = [A.alloc(D) for _ in range(2)]; bxt = [P.buf() for _ in range(2)]
        sm1 = [A.alloc(2) for _ in range(2)]; bsm1 = [P.buf() for _ in range(2)]
        junk = A.alloc(D, BF16); bjunk = P.buf("junk")
        x1t = [A.alloc(D) for _ in range(2)]; bx1 = [P.buf() for _ in range(2)]
        h2f = [A.alloc(D) for _ in range(2)]; bh2f = [P.buf() for _ in range(2)]
        h2b = [A.alloc(D, BF16) for _ in range(2)]; bh2b = [P.buf() for _ in range(2)]
        h2T = [A.alloc(8 * 128) for _ in range(2)]; bh2T = [P.buf() for _ in range(2)]
        def op_a(t):
            gt = sq * 16 + t
            i = gt % 2
            pa, pb2 = (0, 1) if i == 0 else (2, 3)
            dma("sp", xt[i], x_d[gt * 128:(gt + 1) * 128, :], writes=[bxt[i]])
            for half, pb in ((0, pa), (1, pb2)):
                for c in range(8):
                    mm(PS(pb), mixT3[:, c, t * 128:(t + 1) * 128], wout3[:, c, half * 512:(half + 1) * 512], c == 0, c == 7,
                       [bmix[c], bwout], [pbuf[pb]])
                tt("dve", x1t[i][:, half * 512:(half + 1) * 512], xt[i][:, half * 512:(half + 1) * 512], PS(pb), ALU.add,
                   [bxt[i], pbuf[pb]], [bx1[i]])
            dma("sp", x1s[gt * 128:(gt + 1) * 128, :], x1t[i], reads=[bx1[i]], writes=[b_x1s[gt]])
            act(junk, x1t[i], AF.Square, [bx1[i]], [bjunk, bsm1[i]], accum=sm1[i][:, 0:1])
            rsqrt_mean(sm1[i][:, 0:1], sm1[i][:, 0:1], D, [bsm1[i], bC], [bsm1[i]])
            stt("dve", h2f[i], x1t[i], sm1[i][:, 0:1], g2bc, ALU.mult, ALU.mult, [bx1[i], bsm1[i], bC], [bh2f[i]])
            cp("pool", h2b[i], h2f[i], [bh2f[i]], [bh2b[i]])
            dma("sp", h2s[gt * 128:(gt + 1) * 128, :], h2b[i], reads=[bh2b[i]], writes=[b_h2s[gt]])

        def op_b(t):
            gt = sq * 16 + t
            i = gt % 2
            for c in range(8):
                pbt = 4 + c // 4
                tr(PS(pbt)[:, (c % 4) * 128:(c % 4 + 1) * 128], h2f[i][:, c * 128:(c + 1) * 128], ident32, [bh2f[i], bC],
                   [pbuf[pbt]])
            cp("act", h2T[i][:, 0:512], PS(4), [pbuf[4]], [bh2T[i]])
            cp("act", h2T[i][:, 512:1024], PS(5), [pbuf[5]], [bh2T[i]])
            pl = rot("L", [6, 7])
            for c in range(8):
                mm(PS(pl)[:, 0:36], h2T[i][:, c * 128:(c + 1) * 128], wr3[:, c, :], c == 0, c == 7, [bh2T[i], bC], [pbuf[pl]])
            tt("dve", Lall[:, gt * 36:(gt + 1) * 36], PS(pl)[:, 0:36], brbc, ALU.add, [pbuf[pl], bC], [bL])

        op_a(0)
        for t in range(16):
            if t + 1 < 16:
                op_a(t + 1)
            op_b(t)

    P.barrier()
    A.top = top_persist
    L3 = Lall.rearrange("p (t n) -> p t n", n=36)

    def al(n, dt=F32):
        return A.alloc(n, dt)
    bR = P.buf("route")
    gate1 = al(NT); gate2 = al(NT); d1i = al(NT, I32); d2i = al(NT, I32)
    top_route = A.top
    gmax = al(NT); goh = al(NT * 4); gexp = al(NT * 4); gsum = al(NT); wgt = al(NT)
    sel = al(NT * 8); tmp8 = al(NT * 8); m1 = al(NT); m2 = al(NT); oh1 = al(NT * 8); oh2 = al(NT * 8)
    dd = al(NT)
    A1 = al(NT * 32); A2 = al(NT * 32); Aall = al(NT * 32, BF16); pos = al(NT * 32); tot = al(NT * 32)
    cum = al(NT * 32); tmp32 = al(NT * 32); valid = al(NT * 32)
    d1f = al(NT); d2f = al(NT)
    RW = [bL, bR, bC]

    def v3(ap, n):
        return ap.rearrange("p (t n) -> p t n", n=n)

    def bc(ap, n):
        return ap.unsqueeze(2).to_broadcast([128, NT, n])

    P.op("dve", lambda e: e.tensor_reduce(out=gmax, in_=L3[:, :, 0:4], axis=AX.X, op=ALU.max), RW, RW)
    tt("dve", v3(goh, 4), L3[:, :, 0:4], bc(gmax, 4), ALU.is_equal, RW, RW)
    tt("dve", v3(gexp, 4), L3[:, :, 0:4], bc(gmax, 4), ALU.subtract, RW, RW)
    act(gexp, gexp, AF.Exp, RW, RW)
    P.op("dve", lambda e: e.tensor_reduce(out=gsum, in_=v3(gexp, 4), axis=AX.X, op=ALU.add), RW, RW)
    P.op("dve", lambda e: e.reciprocal(out=wgt, in_=gsum), RW, RW)
    for g in range(4):
        src = L3[:, :, 4 + 8 * g:12 + 8 * g]
        gsel = v3(goh, 4)[:, :, g:g + 1].to_broadcast([128, NT, 8])
        if g == 0:
            tt("dve", v3(sel, 8), src, gsel, ALU.mult, RW, RW)
        else:
            tt("dve", v3(tmp8, 8), src, gsel, ALU.mult, RW, RW)
            tt("dve", sel, sel, tmp8, ALU.add, RW, RW)
    P.op("dve", lambda e: e.tensor_reduce(out=m1, in_=v3(sel, 8), axis=AX.X, op=ALU.max), RW, RW)
    tt("dve", v3(oh1, 8), v3(sel, 8), bc(m1, 8), ALU.is_equal, RW, RW)
    stt("dve", tmp8, oh1, -1e30, sel, ALU.mult, ALU.add, RW, RW)
    P.op("dve", lambda e: e.tensor_reduce(out=m2, in_=v3(tmp8, 8), axis=AX.X, op=ALU.max), RW, RW)
    tt("dve", v3(oh2, 8), v3(tmp8, 8), bc(m2, 8), ALU.is_equal, RW, RW)
    tt("dve", dd, m2, m1, ALU.subtract, RW, RW)
    act(dd, dd, AF.Exp, RW, RW)
    ts("dve", dd, dd, 1.0, None, ALU.add, None, RW, RW)
    P.op("dve", lambda e: e.reciprocal(out=dd, in_=dd), RW, RW)
    tt("dve", gate1, wgt, dd, ALU.mult, RW, RW)
    tt("dve", gate2, wgt, gate1, ALU.subtract, RW, RW)
    for (Ak, ohk) in ((A1, oh1), (A2, oh2)):
        tt("dve", Ak.rearrange("p (t g j) -> p t g j", g=4, j=8),
           v3(goh, 4).unsqueeze(3).to_broadcast([128, NT, 4, 8]),
           v3(ohk, 8).unsqueeze(2).to_broadcast([128, NT, 4, 8]), ALU.mult, RW, RW)
    tt("dve", Aall, A1, A2, ALU.add, RW, RW)
    for hf in range(2):
        sl = slice(hf * 512, (hf + 1) * 512)
        mm(PS(hf), C["tri_bf"], Aall[:, sl], True, True, RW, [pbuf[hf]])
        mm(PS(2 + hf), ones_bf, Aall[:, sl], True, True, RW, [pbuf[2 + hf]])
        cp("dve", pos[:, sl], PS(hf), [pbuf[hf]], RW)
        cp("dve", tot[:, sl], PS(2 + hf), [pbuf[2 + hf]], RW)
    P.op("dve", lambda e: e.memset(cum[:, 0:32], 0.0), RW, RW)
    for t in range(1, NT):
        tt("dve", cum[:, t * 32:(t + 1) * 32], cum[:, (t - 1) * 32:t * 32], tot[:, (t - 1) * 32:t * 32], ALU.add, RW, RW)
    tt("dve", pos, pos, cum, ALU.add, RW, RW)
    ts("dve", valid, pos, float(CAP), None, ALU.is_lt, None, RW, RW)
    tt("dve", v3(pos, 32), v3(pos, 32), C["ebase"].unsqueeze(1).to_broadcast([128, NT, 32]), ALU.add, RW, RW)
    ts("dve", pos, pos, float(-TRASH), None, ALU.add, None, RW, RW)
    tt("dve", pos, pos, valid, ALU.mult, RW, RW)
    for (Ak, dkf, dki) in ((A1, d1f, d1i), (A2, d2f, d2i)):
        tt("dve", tmp32, pos, Ak, ALU.mult, RW, RW)
        P.op("dve", lambda e, o_=dkf: e.tensor_reduce(out=o_, in_=v3(tmp32, 32), axis=AX.X, op=ALU.add), RW, RW)
        ts("dve", dkf, dkf, float(TRASH), None, ALU.add, None, RW, RW)
        cp("dve", dki, dkf, RW, RW)

    P.barrier()
    A.top = top_route
    NH = 6
    b_sc = [P.buf() for _ in range(2 * NT)]
    b_sc_used = []
    _hs_top = A.top
    hsb = [A.alloc(D, BF16) for _ in range(NH)]; bhsb = [P.buf() for _ in range(NH)]
    for gt in range(NT):
        i = gt % NH
        dma("sp", hsb[i], h2s[gt * 128:(gt + 1) * 128, :], reads=[b_h2s[gt]], writes=[bhsb[i]])
        for dki in (d1i, d2i):
            P.op("pool", lambda e, s_=hsb[i], o_=dki[:, gt:gt + 1]: e.indirect_dma_start(
                out=xg, out_offset=bass.IndirectOffsetOnAxis(ap=o_, axis=0), in_=s_, in_offset=None),
                [bhsb[i], bR] + b_xgz, [b_sc[len(b_sc_used)]], dma=True)
            b_sc_used.append(1)

    A.top = _hs_top
    XT = [A.alloc(8 * CAP, BF16) for _ in range(2)]; bXT = [P.buf() for _ in range(2)]
    assert A.top >= _hs_top + NH * (D // 2)
    wgb = [A.alloc(8 * 512, BF16) for _ in range(2)]; bwg = [P.buf() for _ in range(2)]
    wub = [A.alloc(8 * 512, BF16) for _ in range(2)]; bwu = [P.buf() for _ in range(2)]
    wdb = [A.alloc(4 * D, BF16) for _ in range(2)]; bwd = [P.buf() for _ in range(2)]
    stage = [[A.alloc(4096) for _ in range(3)] for _ in range(2)]; bstg = [[P.buf() for _ in range(3)] for _ in range(2)]
    NST = CAP // 128
    xgt = [A.alloc(NST * D, BF16) for _ in range(2)]; bxgt = [P.buf() for _ in range(2)]
    sg = [A.alloc(CAP) for _ in range(2)]; bsg = [P.buf() for _ in range(2)]
    hidT = [A.alloc(4 * CAP, BF16) for _ in range(2)]; bhid = [P.buf() for _ in range(2)]
    yt = [A.alloc(D, BF16) for _ in range(3)]; byt = [P.buf() for _ in range(3)]

    b_ys = []

    def loads(ex):
        k = ex % 2
        dma("sp", stage[k][0].rearrange("p (c n) -> p c n", c=8), wg_d[ex].rearrange("(c p) n -> p c n", p=128), writes=[bstg[k][0]])
        dma("sp", stage[k][1].rearrange("p (c n) -> p c n", c=8), wu_d[ex].rearrange("(c p) n -> p c n", p=128), writes=[bstg[k][1]])
        dma("sp", stage[k][2].rearrange("p (c n) -> p c n", c=4), wd_d[ex].rearrange("(c p) n -> p c n", p=128), writes=[bstg[k][2]])

    def load_x(ex):
        i = ex % 2
        dma("sp", xgt[i].rearrange("p (s d) -> p s d", s=NST), xg[ex * CAP:(ex + 1) * CAP, :].rearrange("(s p) d -> p s d", p=128),
            reads=b_sc, writes=[bxgt[i]])

    def casts_gu(ex):
        i = ex % 2
        cp("act", wgb[i][:, 0:2048], stage[i][0][:, 0:2048], [bstg[i][0]], [bwg[i]])
        cp("dve", wgb[i][:, 2048:4096], stage[i][0][:, 2048:4096], [bstg[i][0]], [bwg[i]])
        cp("dve", wub[i][:, 0:2048], stage[i][1][:, 0:2048], [bstg[i][1]], [bwu[i]])
        cp("act", wub[i][:, 2048:4096], stage[i][1][:, 2048:4096], [bstg[i][1]], [bwu[i]])

    def cast_d(ex):
        i = ex % 2
        cp("pool", wdb[i][:, 0:2560], stage[i][2][:, 0:2560], [bstg[i][2]], [bwd[i]])
        cp("act", wdb[i][:, 2560:3328], stage[i][2][:, 2560:3328], [bstg[i][2]], [bwd[i]])
        cp("dve", wdb[i][:, 3328:4096], stage[i][2][:, 3328:4096], [bstg[i][2]], [bwd[i]])

    loads(0); load_x(0); loads(1); load_x(1); casts_gu(0); cast_d(0)
    for ex in range(NE):
        i = ex % 2
        XT3 = XT[i].rearrange("p (c s) -> p c s", c=8)
        for s4 in range(NST):
            pb = rot("xtr", [0, 1])
            for c in range(8):
                tr(PS(pb, BF16)[:, c * 128:(c + 1) * 128], xgt[i][:, s4 * D + c * 128:s4 * D + (c + 1) * 128], ident_bf,
                   [bxgt[i], bC], [pbuf[pb]])
            cp("act" if s4 % 2 else "dve", XT3[:, :, s4 * 128:(s4 + 1) * 128], PS(pb, BF16).rearrange("p (c t) -> p c t", c=8),
               [pbuf[pb]], [bXT[i]])
        if ex + 2 < NE:
            load_x(ex + 2)
        wg3 = wgb[i].rearrange("p (c n) -> p c n", c=8)
        wu3 = wub[i].rearrange("p (c n) -> p c n", c=8)
        wd3 = wdb[i].rearrange("p (c n) -> p c n", c=4)
        for fc in range(4):
            pG = rot("G", [2, 3])
            pUp = rot("Up", [4, 5])
            for c in range(8):
                mm(PS(pG)[:, 0:CAP], wg3[:, c, fc * 128:(fc + 1) * 128], XT3[:, c, :], c == 0, c == 7, [bwg[i], bXT[i]], [pbuf[pG]])
            for c in range(8):
                mm(PS(pUp)[:, 0:CAP], wu3[:, c, fc * 128:(fc + 1) * 128], XT3[:, c, :], c == 0, c == 7, [bwu[i], bXT[i]], [pbuf[pUp]])
            isg = rot("sg", [0, 1])
            act(sg[isg], PS(pG)[:, 0:CAP], AF.Silu, [pbuf[pG]], [bsg[isg]])
            tt("dve", hidT[i][:, fc * CAP:(fc + 1) * CAP], sg[isg], PS(pUp)[:, 0:CAP], ALU.mult, [bsg[isg], pbuf[pUp]], [bhid[i]])
        for s4 in range(NST):
            iy = rot("yt", [0, 1, 2])
            for half in range(2):
                pY = rot("Y", [6, 7])
                for fc in range(4):
                    mm(PS(pY), hidT[i][:, fc * CAP + s4 * 128:fc * CAP + (s4 + 1) * 128], wd3[:, fc, half * 512:(half + 1) * 512],
                       fc == 0, fc == 3, [bhid[i], bwd[i]], [pbuf[pY]])
                cp("act" if half else "dve", yt[iy][:, half * 512:(half + 1) * 512], PS(pY),
                   [pbuf[pY]], [byt[iy]])
            r0 = ex * CAP + s4 * 128
            b_ys.append(P.buf())
            dma("sp", ybuf[r0:r0 + 128, :], yt[iy], reads=[byt[iy]], writes=[b_ys[-1]])
        if ex + 1 < NE:
            cast_d(ex + 1)
            casts_gu(ex + 1)
        if ex + 2 < NE:
            loads(ex + 2)

    P.barrier()
    A.top = top_route
    NB = 4
    gfbc = A.alloc(D); bgf = P.buf()
    dma("sp", gfbc, gf_d, writes=[bgf])
    x1c = [A.alloc(D) for _ in range(NB)]; bx1c = [P.buf() for _ in range(NB)]
    Y1 = [A.alloc(D, BF16) for _ in range(NB)]; bY1 = [P.buf() for _ in range(NB)]
    Y2 = [A.alloc(D, BF16) for _ in range(NB)]; bY2 = [P.buf() for _ in range(NB)]
    oo = [A.alloc(D) for _ in range(NB)]; boo = [P.buf() for _ in range(NB)]
    jk2 = A.alloc(D, BF16); bjk2 = P.buf()
    smf = [A.alloc(2) for _ in range(NB)]; bsmf = [P.buf() for _ in range(NB)]

    def cload(gt):
        i = gt % NB
        dma("sp", x1c[i], x1s[gt * 128:(gt + 1) * 128, :], reads=[b_x1s[gt]], writes=[bx1c[i]])
        for (Yk, bYk, dki) in ((Y1, bY1, d1i), (Y2, bY2, d2i)):
            P.op("pool", lambda e, o_=Yk[i], x_=dki[:, gt:gt + 1]: e.indirect_dma_start(
                out=o_, out_offset=None, in_=ybuf, in_offset=bass.IndirectOffsetOnAxis(ap=x_, axis=0)),
                [b_ybuf, bR] + b_ys, [bYk[i]], dma=True)

    for gt in range(NB - 1):
        cload(gt)
    for gt in range(NT):
        i = gt % NB
        if gt + NB - 1 < NT:
            cload(gt + NB - 1)
        stt("dve", x1c[i], Y1[i], gate1[:, gt:gt + 1], x1c[i], ALU.mult, ALU.add, [bY1[i], bR, bx1c[i]], [bx1c[i]])
        stt("dve", x1c[i], Y2[i], gate2[:, gt:gt + 1], x1c[i], ALU.mult, ALU.add, [bY2[i], bR, bx1c[i]], [bx1c[i]])
        act(jk2, x1c[i], AF.Square, [bx1c[i]], [bjk2, bsmf[i]], accum=smf[i][:, 0:1])
        rsqrt_mean(smf[i][:, 0:1], smf[i][:, 0:1], D, [bsmf[i], bC], [bsmf[i]])
        act(oo[i], x1c[i], AF.Copy, [bx1c[i], bsmf[i]], [boo[i]], scale=smf[i][:, 0:1])
        tt("pool" if gt % 3 else "dve", oo[i], oo[i], gfbc, ALU.mult, [boo[i], bgf], [boo[i]])
        dma("sp", out_d[gt * 128:(gt + 1) * 128, :], oo[i], reads=[boo[i]], writes=[b_out])
    P.final_wait("sp")
    P.emit(st)
    st.close()
    return nc, P


_CACHE = {}


def kernel(x, norm1_g, w_in, attn_norm_g, hgrn_gamma, hgrn_norm_g, w_out, norm2_g,
           w_group, b_group, w_router, b_router, w_gate, w_up, w_down, norm_f_g):
    f32 = np.float32
    x = np.asarray(x, f32)
    if "nc" not in _CACHE:
        _CACHE["nc"] = build()
    nc, _ = _CACHE["nc"]
    cons = host_consts()

    def bc128(v):
        return np.ascontiguousarray(np.broadcast_to(np.asarray(v, f32).reshape(1, -1), (128, v.size)))

    def fm(v, c):
        return np.ascontiguousarray(np.asarray(v, f32).reshape(c, 128).T)

    wr = np.concatenate([np.asarray(w_group, f32)[0], np.asarray(w_router, f32)[0]], axis=1)
    br = np.concatenate([np.asarray(b_group, f32)[0], np.asarray(b_router, f32)[0]], axis=0)
    shared = {
        "w_in": np.ascontiguousarray(np.asarray(w_in, f32)[0]),
        "w_out": np.ascontiguousarray(np.asarray(w_out, f32)[0]),
        "g1bc": bc128(np.asarray(norm1_g, f32)[0]),
        "g2bc": bc128(np.asarray(norm2_g, f32)[0]),
        "gfbc": bc128(np.asarray(norm_f_g, f32)),
        "ga": fm(np.asarray(attn_norm_g, f32)[0], 4),
        "gh": fm(np.asarray(hgrn_norm_g, f32)[0], 4),
        "gam": np.ascontiguousarray(np.concatenate([fm(np.asarray(hgrn_gamma, f32)[0], 4),
                                                    fm(np.asarray(hgrn_gamma, f32)[1], 4)], axis=1)),
        "wr": np.ascontiguousarray(wr.reshape(8, 128, 36).transpose(1, 0, 2)),
        "brbc": bc128(br),
        "w_gate": np.ascontiguousarray(np.asarray(w_gate, f32)[0]),
        "w_up": np.ascontiguousarray(np.asarray(w_up, f32)[0]),
        "w_down": np.ascontiguousarray(np.asarray(w_down, f32)[0]),
    }
    for k, v in cons.items():
        shared["c_" + k] = v
    xs = x.reshape(NCORES, T, D)
    in_maps = [dict(shared, x=np.ascontiguousarray(xs[i])) for i in range(NCORES)]
    res = run_bass_kernel_spmd(nc, in_maps, core_ids=list(range(NCORES)))
    out = np.stack([np.asarray(r["out"], f32).reshape(T, D) for r in res.results], axis=0)
    return out.reshape(16, S, D)
```
